# Optimizing a Trainium2 kernel written in Bass

```python
import math
import jax, jax.numpy as jnp
from jax import lax
import numpy as np

D_MODEL = 1024
BATCH = 8
SEQ = 2048
DEPTH = 2

GRID_W = 64
CTX_LEN = 256

N_BRANCH = 4
BRANCH_W = 512
GDN_HEADS = 4
GDN_DK = 128
GDN_DV = 128
GDN_CHUNK = 64
SHORT_CONV = 5
CONF_KW = 31
CONF_GROUPS = 4
FFT_GROUPS = 4
SGU_CHUNK = 128
SGU_GROUPS = 4
MOE_GROUPS = 4
MOE_PER_GROUP = 4
N_EXPERTS = MOE_GROUPS * MOE_PER_GROUP
MOE_TOPK = 2
EXPERT_HIDDEN = 512

DN_ALPHA = (2 * DEPTH) ** 0.25
DN_BETA = (8 * DEPTH) ** -0.25
LN_EPS = 1e-6

OFF_Q = 0
OFF_K = OFF_Q + GDN_HEADS * GDN_DK
OFF_V = OFF_K + GDN_HEADS * GDN_DK
OFF_Z = OFF_V + GDN_HEADS * GDN_DV
OFF_A = OFF_Z + GDN_HEADS * GDN_DV
OFF_B = OFF_A + 2 * GDN_HEADS
OFF_CONF = OFF_B + 2 * GDN_HEADS
OFF_FFT = OFF_CONF + 2 * BRANCH_W
OFF_SGU = OFF_FFT + BRANCH_W
OFF_GATE = OFF_SGU + 2 * BRANCH_W
N_IN = OFF_GATE + N_BRANCH * D_MODEL

kernel_name = 'hybrid_gdn_conformer_fnet_sgu_hmoe'


def layer_norm(x, eps=LN_EPS):
    xf = x.astype(jnp.float32)
    mu = jnp.mean(xf, -1, keepdims=True)
    var = jnp.mean(jnp.square(xf - mu), -1, keepdims=True)
    return ((xf - mu) * lax.rsqrt(var + eps)).astype(x.dtype)


def affine_ln(x, g, b):
    return layer_norm(x) * g + b


def group_ln(x, g, b, groups):
    shp = x.shape
    y = layer_norm(x.reshape(*shp[:-1], groups, shp[-1] // groups)).reshape(shp)
    return y * g + b


def rms_norm(x, w, eps=LN_EPS):
    xf = x.astype(jnp.float32)
    return xf * lax.rsqrt(jnp.mean(jnp.square(xf), -1, keepdims=True) + eps) * w


def l2_normalize(x, eps=LN_EPS):
    xf = x.astype(jnp.float32)
    return xf * lax.rsqrt(jnp.sum(jnp.square(xf), -1, keepdims=True) + eps)


def modulate(x, shift, scale):
    return layer_norm(x) * (1 + scale) + shift


def depthwise_conv(x, w):
    k, ch = w.shape
    return lax.conv_general_dilated(x, w[:, None, :].astype(x.dtype), window_strides=(1,),
                                    padding=[(k // 2, k // 2)],
                                    dimension_numbers=('NWC', 'WIO', 'NWC'),
                                    feature_group_count=ch)


def sincos_2d(n, d):
    rows = n // GRID_W
    row = jnp.broadcast_to(jnp.arange(rows, dtype=jnp.float32)[:, None], (rows, GRID_W)).reshape(-1)
    col = jnp.broadcast_to(jnp.arange(GRID_W, dtype=jnp.float32)[None, :], (rows, GRID_W)).reshape(-1)
    quarter = d // 4
    omega = 1.0 / (10000.0 ** (jnp.arange(quarter, dtype=jnp.float32) / quarter))

    def enc(pos):
        ang = pos[:, None] * omega[None, :]
        return jnp.concatenate([jnp.sin(ang), jnp.cos(ang)], -1)

    return jnp.concatenate([enc(row), enc(col)], -1)


def gdn_chunk_scan(q, k, v, g, beta, s0):
    bsz, t, h, dk = q.shape
    dv = v.shape[-1]
    cs = GDN_CHUNK
    n = t // cs

    def to_chunks(a):
        a = a.reshape(bsz, n, cs, h, *a.shape[3:])
        return jnp.moveaxis(a, 3, 1)

    q, k, v, g, beta = (to_chunks(a) for a in (q, k, v, g, beta))
    gc = jnp.cumsum(g, axis=-1)
    incl = jnp.tril(jnp.ones((cs, cs), bool))
    strict = jnp.tril(jnp.ones((cs, cs), bool), -1)
    decay = jnp.exp(jnp.where(incl, gc[..., :, None] - gc[..., None, :], -jnp.inf))
    kb = k * beta[..., None]
    kk = jnp.einsum('bhnid,bhnjd->bhnij', kb, k) * decay
    a_mat = jnp.eye(cs, dtype=jnp.float32) + jnp.where(strict, kk, 0.0)
    rhs = jnp.concatenate([v * beta[..., None], kb * jnp.exp(gc)[..., None]], -1)
    sol = lax.linalg.triangular_solve(a_mat, rhs, left_side=True, lower=True, unit_diagonal=True)
    u, w = sol[..., :dv], sol[..., dv:]
    attn = jnp.einsum('bhnid,bhnjd->bhnij', q, k) * decay
    qg = q * jnp.exp(gc)[..., None]
    g_last = gc[..., -1]
    kd = k * jnp.exp(g_last[..., None] - gc)[..., None]

    def step(s, xs):
        w_n, u_n, qg_n, kd_n, attn_n, gl_n = xs
        v_new = u_n - jnp.einsum('bhcd,bhde->bhce', w_n, s)
        o_n = jnp.einsum('bhcd,bhde->bhce', qg_n, s) + jnp.einsum('bhij,bhje->bhie', attn_n, v_new)
        s = s * jnp.exp(gl_n)[..., None, None] + jnp.einsum('bhcd,bhce->bhde', kd_n, v_new)
        return s, o_n

    xs = tuple(jnp.moveaxis(a, 2, 0) for a in (w, u, qg, kd, attn, g_last))
    s_fin, o = lax.scan(step, s0, xs)
    o = o.transpose(1, 0, 3, 2, 4).reshape(bsz, t, h, dv)
    return o, s_fin


def gdn_inputs(p, conv_w, a_log, dt_bias):
    bsz, t, _ = p.shape
    qkv = jax.nn.silu(depthwise_conv(p[..., OFF_Q:OFF_Z], conv_w)).astype(jnp.float32)
    q = l2_normalize(qkv[..., OFF_Q:OFF_K].reshape(bsz, t, GDN_HEADS, GDN_DK)) * (GDN_DK ** -0.5)
    k = l2_normalize(qkv[..., OFF_K:OFF_V].reshape(bsz, t, GDN_HEADS, GDN_DK))
    v = qkv[..., OFF_V:OFF_Z].reshape(bsz, t, GDN_HEADS, GDN_DV)
    a = p[..., OFF_A:OFF_B].astype(jnp.float32).reshape(bsz, t, 2, GDN_HEADS)
    b = p[..., OFF_B:OFF_CONF].astype(jnp.float32).reshape(bsz, t, 2, GDN_HEADS)
    g = -jnp.exp(a_log.astype(jnp.float32)) * jax.nn.softplus(a + dt_bias.astype(jnp.float32))
    beta = jax.nn.sigmoid(b)
    return q, k, v, g, beta


def gdn_bidirectional(inputs, s0_f, s0_b):
    q, k, v, g, beta = inputs
    o_f, s_f = gdn_chunk_scan(q, k, v, g[:, :, 0], beta[:, :, 0], s0_f)
    flip = lambda a: jnp.flip(a, axis=1)
    o_b, s_b = gdn_chunk_scan(flip(q), flip(k), flip(v), flip(g[:, :, 1]), flip(beta[:, :, 1]), s0_b)
    return o_f + flip(o_b), s_f, s_b


def gdn_branch(o, p, norm_w):
    bsz, t = p.shape[:2]
    z = p[..., OFF_Z:OFF_A].astype(jnp.float32).reshape(bsz, t, GDN_HEADS, GDN_DV)
    y = rms_norm(o, norm_w.astype(jnp.float32)) * jax.nn.silu(z)
    return y.reshape(bsz, t, GDN_HEADS * GDN_DV).astype(p.dtype)


def conformer_branch(p, dw_w, dw_b, ln_g, ln_b):
    a, gt = jnp.split(p[..., OFF_CONF:OFF_FFT], 2, axis=-1)
    h = depthwise_conv(a * jax.nn.sigmoid(gt), dw_w) + dw_b
    return jax.nn.silu(group_ln(h, ln_g, ln_b, CONF_GROUPS))


def fourier_branch(p):
    bsz, t = p.shape[:2]
    h = p[..., OFF_FFT:OFF_SGU].astype(jnp.float32).reshape(bsz, t, FFT_GROUPS, BRANCH_W // FFT_GROUPS)
    y = jnp.fft.fft2(h, axes=(1, 3), norm='ortho').real
    return y.reshape(bsz, t, BRANCH_W).astype(p.dtype)


def sgu_branch(p, ln_g, ln_b, ws, bs):
    bsz, t = p.shape[:2]
    u, v = jnp.split(jax.nn.gelu(p[..., OFF_SGU:OFF_GATE]), 2, axis=-1)
    v = group_ln(v, ln_g, ln_b, SGU_GROUPS)
    vc = v.reshape(bsz, t // SGU_CHUNK, SGU_CHUNK, SGU_GROUPS, BRANCH_W // SGU_GROUPS)
    s = jnp.einsum('gpq,bnqgc->bnpgc', ws, vc) + bs.T[None, None, :, :, None]
    return u * s.reshape(bsz, t, BRANCH_W)


def token_mix_out(p, o_gdn, gdn_norm_w, conf_dw_w, conf_dw_b, conf_ln_g, conf_ln_b,
                  sgu_ln_g, sgu_ln_b, sgu_ws, sgu_bs, w_branch, w_out, b_out):
    bsz, t = p.shape[:2]
    branches = jnp.stack([gdn_branch(o_gdn, p, gdn_norm_w),
                          conformer_branch(p, conf_dw_w, conf_dw_b, conf_ln_g, conf_ln_b),
                          fourier_branch(p),
                          sgu_branch(p, sgu_ln_g, sgu_ln_b, sgu_ws, sgu_bs)], axis=2)
    proj = jnp.einsum('btiw,iwd->btid', branches, w_branch)
    gates = jax.nn.sigmoid(p[..., OFF_GATE:].reshape(bsz, t, N_BRANCH, D_MODEL))
    merged = jnp.sum(gates * proj, axis=2)
    return merged @ w_out + b_out


def hier_moe(h, rg_w, rg_b, re_w, re_b, w_gate, w_up, w_down):
    shp = h.shape
    tok = h.reshape(-1, shp[-1])
    glog = (tok @ rg_w + rg_b).astype(jnp.float32)
    gprob = jax.nn.softmax(glog, axis=-1)
    gsel = jnp.argmax(glog, axis=-1)
    gp = jnp.max(gprob, axis=-1, keepdims=True)
    elog = (tok @ re_w + re_b).astype(jnp.float32).reshape(-1, MOE_GROUPS, MOE_PER_GROUP)
    elog_sel = jnp.sum(elog * jax.nn.one_hot(gsel, MOE_GROUPS, dtype=jnp.float32)[:, :, None], axis=1)
    top_v, top_i = lax.top_k(elog_sel, MOE_TOPK)
    top_w = jax.nn.softmax(top_v, axis=-1) * gp
    eidx = gsel[:, None] * MOE_PER_GROUP + top_i
    comb = jnp.sum(jax.nn.one_hot(eidx, N_EXPERTS, dtype=jnp.float32) * top_w[..., None], axis=1)
    out = jnp.zeros(tok.shape, jnp.float32)
    for e in range(N_EXPERTS):
        hid = jax.nn.silu(tok @ w_gate[e]) * (tok @ w_up[e])
        out = out + comb[:, e:e + 1] * (hid @ w_down[e])
    return out.reshape(shp).astype(h.dtype)


def setup_inputs(seed: int = 0) -> dict:
    key = jax.random.key(seed)
    keys = jax.random.split(key, 48)
    counter = [0]

    def nk():
        k = keys[counter[0]]
        counter[0] += 1
        return k

    def nrm(shape, scale):
        return jax.random.normal(nk(), shape, jnp.float32) * scale

    L, D = DEPTH, D_MODEL
    x = nrm((BATCH, SEQ, D), 1.0)
    c = nrm((BATCH, D), 1.0)
    ctx = nrm((BATCH, CTX_LEN, D), 1.0)
    c_ctx = nrm((D,), 1.0)
    w_mod = nrm((L, D, 6 * D), 0.5 * D ** -0.5)
    b_mod = nrm((L, 6 * D), 0.02)
    w_in = nrm((L, D, N_IN), D ** -0.5)
    b_in = nrm((L, N_IN), 0.02)
    gdn_conv_w = nrm((L, SHORT_CONV, OFF_Z), SHORT_CONV ** -0.5)
    gdn_a_log = jnp.log(jax.random.uniform(nk(), (L, 2, GDN_HEADS), jnp.float32, 1.0, 16.0))
    dt = jnp.exp(jax.random.uniform(nk(), (L, 2, GDN_HEADS), jnp.float32, math.log(1e-3), math.log(1e-1)))
    gdn_dt_bias = dt + jnp.log(-jnp.expm1(-dt))
    gdn_norm_w = 1.0 + nrm((L, GDN_DV), 0.02)
    conf_dw_w = nrm((L, CONF_KW, BRANCH_W), CONF_KW ** -0.5)
    conf_dw_b = nrm((L, BRANCH_W), 0.02)
    conf_ln_g = 1.0 + nrm((L, BRANCH_W), 0.02)
    conf_ln_b = nrm((L, BRANCH_W), 0.02)
    sgu_ln_g = 1.0 + nrm((L, BRANCH_W), 0.02)
    sgu_ln_b = nrm((L, BRANCH_W), 0.02)
    sgu_ws = nrm((L, SGU_GROUPS, SGU_CHUNK, SGU_CHUNK), 0.5 * SGU_CHUNK ** -0.5)
    sgu_bs = 1.0 + nrm((L, SGU_GROUPS, SGU_CHUNK), 0.02)
    w_branch = nrm((L, N_BRANCH, BRANCH_W, D), DN_BETA * BRANCH_W ** -0.5)
    w_out = nrm((L, D, D), DN_BETA * D ** -0.5)
    b_out = nrm((L, D), 0.02)
    ln1_g = 1.0 + nrm((L, D), 0.02)
    ln1_b = nrm((L, D), 0.02)
    ln2_g = 1.0 + nrm((L, D), 0.02)
    ln2_b = nrm((L, D), 0.02)
    router_group_w = nrm((L, D, MOE_GROUPS), D ** -0.5)
    router_group_b = nrm((L, MOE_GROUPS), 0.01)
    router_expert_w = nrm((L, D, N_EXPERTS), D ** -0.5)
    router_expert_b = nrm((L, N_EXPERTS), 0.01)
    expert_w_gate = nrm((L, N_EXPERTS, D, EXPERT_HIDDEN), D ** -0.5)
    expert_w_up = nrm((L, N_EXPERTS, D, EXPERT_HIDDEN), D ** -0.5)
    expert_w_down = nrm((L, N_EXPERTS, EXPERT_HIDDEN, D), DN_BETA * EXPERT_HIDDEN ** -0.5)
    return {'x': x, 'c': c, 'ctx': ctx, 'c_ctx': c_ctx, 'w_mod': w_mod, 'b_mod': b_mod,
            'w_in': w_in, 'b_in': b_in, 'gdn_conv_w': gdn_conv_w, 'gdn_a_log': gdn_a_log,
            'gdn_dt_bias': gdn_dt_bias, 'gdn_norm_w': gdn_norm_w, 'conf_dw_w': conf_dw_w,
            'conf_dw_b': conf_dw_b, 'conf_ln_g': conf_ln_g, 'conf_ln_b': conf_ln_b,
            'sgu_ln_g': sgu_ln_g, 'sgu_ln_b': sgu_ln_b, 'sgu_ws': sgu_ws, 'sgu_bs': sgu_bs,
            'w_branch': w_branch, 'w_out': w_out, 'b_out': b_out, 'ln1_g': ln1_g, 'ln1_b': ln1_b,
            'ln2_g': ln2_g, 'ln2_b': ln2_b, 'router_group_w': router_group_w,
            'router_group_b': router_group_b, 'router_expert_w': router_expert_w,
            'router_expert_b': router_expert_b, 'expert_w_gate': expert_w_gate,
            'expert_w_up': expert_w_up, 'expert_w_down': expert_w_down}


def reference(x, c, ctx, c_ctx, w_mod, b_mod, w_in, b_in, gdn_conv_w, gdn_a_log, gdn_dt_bias, gdn_norm_w,
              conf_dw_w, conf_dw_b, conf_ln_g, conf_ln_b, sgu_ln_g, sgu_ln_b, sgu_ws, sgu_bs,
              w_branch, w_out, b_out, ln1_g, ln1_b, ln2_g, ln2_b, router_group_w, router_group_b,
              router_expert_w, router_expert_b, expert_w_gate, expert_w_up, expert_w_down):
    bsz, n_lat, d = x.shape
    h_lat = x + sincos_2d(n_lat, d).astype(x.dtype)[None]
    h_ctx = ctx
    s_zero = jnp.zeros((bsz, GDN_HEADS, GDN_DK, GDN_DV), jnp.float32)
    for l in range(DEPTH):
        last = l == DEPTH - 1
        mod_lat = jnp.split((jax.nn.silu(c) @ w_mod[l] + b_mod[l])[:, None, :], 6, axis=-1)
        mod_ctx = jnp.split((jax.nn.silu(c_ctx) @ w_mod[l] + b_mod[l])[None, None, :], 6, axis=-1)
        mix_w = (gdn_norm_w[l], conf_dw_w[l], conf_dw_b[l], conf_ln_g[l], conf_ln_b[l],
                 sgu_ln_g[l], sgu_ln_b[l], sgu_ws[l], sgu_bs[l], w_branch[l], w_out[l], b_out[l])
        moe_w = (router_group_w[l], router_group_b[l], router_expert_w[l], router_expert_b[l],
                 expert_w_gate[l], expert_w_up[l], expert_w_down[l])
        p_lat = modulate(h_lat, mod_lat[0], mod_lat[1]) @ w_in[l] + b_in[l]
        p_ctx = modulate(h_ctx, mod_ctx[0], mod_ctx[1]) @ w_in[l] + b_in[l]
        gdn_c = gdn_inputs(p_ctx, gdn_conv_w[l], gdn_a_log[l], gdn_dt_bias[l])
        o_ctx, s_f, s_b = gdn_bidirectional(gdn_c, s_zero, s_zero)
        gdn_l = gdn_inputs(p_lat, gdn_conv_w[l], gdn_a_log[l], gdn_dt_bias[l])
        o_lat, _, _ = gdn_bidirectional(gdn_l, s_f, s_b)
        y_lat = token_mix_out(p_lat, o_lat, *mix_w)
        h_lat = affine_ln(DN_ALPHA * h_lat + mod_lat[2] * y_lat, ln1_g[l], ln1_b[l])
        m_lat = hier_moe(modulate(h_lat, mod_lat[3], mod_lat[4]), *moe_w)
        h_lat = affine_ln(DN_ALPHA * h_lat + mod_lat[5] * m_lat, ln2_g[l], ln2_b[l])
        if not last:
            y_ctx = token_mix_out(p_ctx, o_ctx, *mix_w)
            h_ctx = affine_ln(DN_ALPHA * h_ctx + mod_ctx[2] * y_ctx, ln1_g[l], ln1_b[l])
            m_ctx = hier_moe(modulate(h_ctx, mod_ctx[3], mod_ctx[4]), *moe_w)
            h_ctx = affine_ln(DN_ALPHA * h_ctx + mod_ctx[5] * m_ctx, ln2_g[l], ln2_b[l])
    return h_lat
```

```python
import contextlib
import numpy as np
import concourse.bass as bass
import concourse.mybir as mybir
from concourse.bass_utils import run_bass_kernel_spmd

F32 = mybir.dt.float32
BF16 = mybir.dt.bfloat16
AF = mybir.ActivationFunctionType
ALU = mybir.AluOpType
AX = mybir.AxisListType


class Buf:
    def __init__(self, k, name, ap, dma_sem=False):
        self.k = k
        self.name = name
        self.ap = ap
        self.w = None
        self.r = []
        self.ds = {}
        self.is_psum = False

    def __getitem__(self, key):
        return View(self, self.ap[key])

    def v(self, ap):
        return View(self, ap)


class View:
    def __init__(self, buf, ap):
        self.buf = buf
        self.ap = ap

    def __getitem__(self, key):
        return View(self.buf, self.ap[key])

    def re(self, s, **kw):
        return View(self.buf, self.ap.rearrange(s, **kw))

    def bc(self, shape):
        return View(self.buf, self.ap.broadcast_to(shape))

    def bitcast(self, dt):
        return View(self.buf, self.ap.bitcast(dt))


class Eng:
    def __init__(self, k, name, e):
        self.k = k
        self.name = name
        self.e = e
        self.sem = k.nc.alloc_semaphore(name="s_" + name)
        self.n = 0
        self.seen = {}


class DmaTicket:
    __slots__ = ("buf", "kind", "val")

    def __init__(self, buf, kind, val):
        self.buf = buf
        self.kind = kind
        self.val = val


class EngTicket:
    __slots__ = ("eng", "val")

    def __init__(self, eng, val):
        self.eng = eng
        self.val = val


class KB:
    def __init__(self, nc):
        self.nc = nc
        self.pe = Eng(self, "pe", nc.tensor)
        self.act = Eng(self, "act", nc.scalar)
        self.dve = Eng(self, "dve", nc.vector)
        self.pool = Eng(self, "pool", nc.gpsimd)
        self.sp = Eng(self, "sp", nc.sync)
        self.engs = [self.pe, self.act, self.dve, self.pool, self.sp]
        self.stack = contextlib.ExitStack()
        self.free_dsems = {"hw": [], "sw": []}
        self.live_dsem = {}
        self.n_dsem = 0
        self.uid = 0
        self.ninst = 0

    def _name(self, name):
        self.uid += 1
        return "%s_%d" % (name, self.uid)

    def sbuf(self, stack, name, shape, dt):
        t = stack.enter_context(self.nc.sbuf_tensor(self._name(name), list(shape), dt))
        b = Buf(self, name, t[tuple(slice(None) for _ in shape)])
        stack.callback(self._release, b)
        return b

    def psum(self, stack, name, shape, dt=F32):
        assert dt == F32
        free = 1
        for d_ in shape[1:]:
            free *= d_
        nb = (free + 511) // 512
        t = stack.enter_context(self.nc.psum_tensor(self._name(name), [128, nb * 512], dt))
        ap = t[0:shape[0], 0:free]
        if len(shape) == 3:
            ap = ap.rearrange("p (a b) -> p a b", a=shape[1])
        elif len(shape) == 4:
            ap = ap.rearrange("p (a b c) -> p a b c", a=shape[1], b=shape[2])
        b = Buf(self, name, ap)
        b.is_psum = True
        stack.callback(self._release, b)
        return b

    def dram(self, name, shape, dt, kind="Internal"):
        t = self.nc.dram_tensor(name, list(shape), dt, kind=kind)
        return Buf(self, name, t.ap())

    def _release(self, b):
        for kind, (sem, n) in b.ds.items():
            self.free_dsems[kind].append((sem, n))
        b.ds = {}
        self.live_dsem.pop(id(b), None)

    def _get_dsem(self, b, kind):
        if kind not in b.ds:
            if self.free_dsems[kind]:
                sem, n = self.free_dsems[kind].pop()
            else:
                self.n_dsem += 1
                sem, n = self.nc.alloc_semaphore(name="d%s_%d" % (kind, self.n_dsem)), 0
            b.ds[kind] = [sem, n]
            self.live_dsem[id(b)] = b
        return b.ds[kind]

    def barrier(self):
        for e in self.engs:
            for f in self.engs:
                if f is not e and f.n > 0:
                    self._wait(e, EngTicket(f, f.n))
            for b in self.live_dsem.values():
                for kind, (sem, n) in b.ds.items():
                    if n > 0:
                        self._wait(e, DmaTicket(b, kind, 16 * n))

    def _wait(self, eng, t):
        if isinstance(t, EngTicket):
            sem, val, key = t.eng.sem, t.val, ("e", t.eng.name)
        else:
            if t.kind not in t.buf.ds:
                return
            sem, n = t.buf.ds[t.kind]
            val = 16 * n
            key = ("d", id(sem))
        if eng.seen.get(key, 0) >= val:
            return
        eng.seen[key] = val
        eng.e.wait_ge(sem, val)
        self.ninst += 1

    def _deps(self, eng, reads, writes, is_pe=False):
        need = []
        for b in reads:
            if b.w is not None:
                need.append(b.w)
            if b.is_psum:
                for t in b.r:
                    if isinstance(t, EngTicket) and t.eng is not eng:
                        need.append(t)
        for b in writes:
            if b.w is not None:
                need.append(b.w)
            for t in b.r:
                need.append(t)
        for t in need:
            if is_pe and isinstance(t, EngTicket) and t.eng is eng:
                continue
            self._wait(eng, t)

    def _mark(self, ticket, reads, writes):
        for b in writes:
            b.w = ticket
            b.r = []
        for b in reads:
            if b in writes:
                continue
            b.r.append(ticket)
            if len(b.r) > 24:
                d = {}
                for t in b.r:
                    key = ("e", t.eng.name) if isinstance(t, EngTicket) else ("d", id(t.buf), t.kind)
                    if key not in d or d[key].val < t.val:
                        d[key] = t
                b.r = list(d.values())

    def op(self, eng, fn, reads, writes):
        rb = [x.buf if isinstance(x, View) else x for x in reads]
        wb = [x.buf if isinstance(x, View) else x for x in writes]
        self._deps(eng, rb, wb, is_pe=(eng is self.pe))
        ins = fn()
        eng.n += 1
        ins.then_inc(eng.sem, 1)
        self.ninst += 1
        self._mark(EngTicket(eng, eng.n), rb, wb)
        return ins

    def dma(self, q, out, in_, **kw):
        ov = out if isinstance(out, View) else out[...]
        iv = in_ if isinstance(in_, View) else in_[...]
        ob, ib = ov.buf, iv.buf
        self._deps(q, [ib], [ob])
        owner = ob
        if str(ob.ap.space).upper().find("DRAM") >= 0 and str(ib.ap.space).upper().find("DRAM") < 0:
            owner = ib
        kind = "sw" if q is self.pool else "hw"
        ds = self._get_dsem(owner, kind)
        ins = q.e.dma_start(out=ov.ap, in_=iv.ap, **kw)
        ds[1] += 1
        ins.then_inc(ds[0], 16)
        self.ninst += 1
        self._mark(DmaTicket(owner, kind, 16 * ds[1]), [ib], [ob])
        return ins

    def mm(self, out, lhsT, rhs, start=True, stop=True, **kw):
        return self.op(self.pe, lambda: self.nc.tensor.matmul(out.ap, lhsT.ap, rhs.ap, start=start, stop=stop, **kw),
                       [lhsT, rhs], [out])

    def actf(self, out, in_, func, bias=None, scale=None, eng=None, **kw):
        reads = [in_]
        args = {}
        if bias is not None:
            if isinstance(bias, View):
                reads.append(bias)
                args["bias"] = bias.ap
            else:
                args["bias"] = bias
        if scale is not None:
            if isinstance(scale, View):
                reads.append(scale)
                args["scale"] = scale.ap
            else:
                args["scale"] = scale
        writes = [out]
        if "accum_out" in kw and kw["accum_out"] is not None:
            writes.append(kw["accum_out"])
            kw["accum_out"] = kw["accum_out"].ap
        return self.op(self.act, lambda: self.nc.scalar.activation(out=out.ap, in_=in_.ap, func=func, **args, **kw),
                       reads, writes)

    def _veng(self, eng):
        eng = eng or self.dve
        return eng, eng.e

    def tt(self, out, in0, in1, op, eng=None):
        eng, e = self._veng(eng)
        return self.op(eng, lambda: e.tensor_tensor(out=out.ap, in0=in0.ap, in1=in1.ap, op=op), [in0, in1], [out])

    def ts(self, out, in0, s1, s2, op0, op1=None, eng=None, accum_out=None):
        eng, e = self._veng(eng)
        reads = [in0]
        a1 = s1
        a2 = s2
        if isinstance(s1, View):
            reads.append(s1)
            a1 = s1.ap
        if isinstance(s2, View):
            reads.append(s2)
            a2 = s2.ap
        kw = {}
        writes = [out]
        if op1 is not None:
            kw["op1"] = op1
        if accum_out is not None:
            kw["accum_out"] = accum_out.ap
            writes.append(accum_out)
        return self.op(eng, lambda: e.tensor_scalar(out=out.ap, in0=in0.ap, scalar1=a1, scalar2=a2, op0=op0, **kw),
                       reads, writes)

    def stt(self, out, in0, scalar, in1, op0, op1, eng=None):
        eng, e = self._veng(None)
        reads = [in0, in1]
        a = scalar
        if isinstance(scalar, View):
            reads.append(scalar)
            a = scalar.ap
        return self.op(eng, lambda: e.scalar_tensor_tensor(out=out.ap, in0=in0.ap, scalar=a, in1=in1.ap, op0=op0, op1=op1),
                       reads, [out])

    def copy(self, out, in_, eng=None):
        eng, e = self._veng(eng)
        if eng is self.act:
            return self.op(eng, lambda: self.nc.scalar.copy(out=out.ap, in_=in_.ap), [in_], [out])
        return self.op(eng, lambda: e.tensor_copy(out=out.ap, in_=in_.ap), [in_], [out])

    def memset(self, out, val, eng=None):
        eng, e = self._veng(eng)
        return self.op(eng, lambda: e.memset(out.ap, val), [], [out])

    def reduce(self, out, in_, op, axis=AX.X, eng=None):
        eng, e = self._veng(eng)
        return self.op(eng, lambda: e.tensor_reduce(out=out.ap, in_=in_.ap, axis=axis, op=op), [in_], [out])

    def recip(self, out, in_, eng=None):
        eng, e = self._veng(eng)
        return self.op(eng, lambda: e.reciprocal(out=out.ap, in_=in_.ap), [in_], [out])

    def finish(self, out_bufs):
        for b in out_bufs:
            if b.w is not None:
                self._wait(self.sp, b.w)
        for e in self.engs:
            if e is not self.sp and e.n > 0:
                self._wait(self.sp, EngTicket(e, e.n))

ExitStack = contextlib.ExitStack
D = 1024
NT = 2304
NTILE = 18
TBLK = [(0, 512), (512, 512), (1024, 512), (1536, 512), (2048, 256)]
SEGS = [(0, 2048), (2048, 256)]
ALPHA = 4 ** 0.25
EPS = 1e-6
O_Q, O_K, O_V, O_Z, O_CA, O_CG, O_F, O_SU, O_SV, O_G, O_AB = 0, 512, 1024, 1536, 2048, 2560, 3072, 3584, 4096, 4608, 8704
NCH = 69
GELU_C = 1.5957691216057308


def seg_of_tile(t):
    return 0 if t < 16 else 1


class Ctx:
    pass


def lockstep(gens):
    gens = list(gens)
    while gens:
        for g_ in list(gens):
            try:
                next(g_)
            except StopIteration:
                gens.remove(g_)


def ln_stats_g(kb, src, xc, junk, small):
    s, nm, ss, rstd = small[:, 0:1], small[:, 1:2], small[:, 2:3], small[:, 3:4]
    kb.actf(junk, src, AF.Identity, accum_out=s)
    yield
    kb.ts(nm, s, -1.0 / D, None, ALU.mult)
    yield
    kb.actf(xc, src, AF.Identity, bias=nm)
    yield
    kb.actf(junk, xc, AF.Square, accum_out=ss)
    yield
    kb.ts(rstd, ss, 1.0 / D, EPS, ALU.mult, ALU.add)
    yield
    kb.actf(rstd, rstd, AF.Sqrt)
    yield
    kb.recip(rstd, rstd)
    yield


def ln_stats(kb, st, src, xc, junk, small):
    s, nm, ss, rstd = small[:, 0:1], small[:, 1:2], small[:, 2:3], small[:, 3:4]
    kb.actf(junk, src, AF.Identity, accum_out=s)
    kb.ts(nm, s, -1.0 / D, None, ALU.mult)
    kb.actf(xc, src, AF.Identity, bias=nm)
    kb.actf(junk, xc, AF.Square, accum_out=ss)
    kb.ts(rstd, ss, 1.0 / D, EPS, ALU.mult, ALU.add)
    kb.actf(rstd, rstd, AF.Sqrt)
    kb.recip(rstd, rstd)
    return rstd


def phase_consts(kb, C, G):
    nc = kb.nc
    st = C.gst
    C.ident = kb.sbuf(st, "ident", [128, 128], F32)
    kb.dma(kb.sp, C.ident, G["ident"])
    C.identb = kb.sbuf(st, "identb", [128, 128], BF16)
    kb.copy(C.identb[:, :], C.ident[:, :])
    C.ones = kb.sbuf(st, "ones", [128, 128], F32)
    kb.memset(C.ones[:, :], 1.0)
    C.onesm = kb.sbuf(st, "onesm", [128, 128], F32)
    kb.memset(C.onesm[:, :], 1.0 / 128)
    C.modT_all = [kb.sbuf(st, "modT%d" % i, [128, 48, 2], F32) for i in range(2)]
    C.onep_all = [kb.sbuf(st, "onep%d" % i, [128, 16, 2], F32) for i in range(2)]
    C.modT, C.onep = C.modT_all[0], C.onep_all[0]
    C.small = kb.sbuf(st, "small", [128, 4], F32)


def phase_h0(kb, C, G):
    with ExitStack() as st:
        xt = [kb.sbuf(st, "xt%d" % i, [128, D], F32) for i in range(2)]
        pt = [kb.sbuf(st, "pt%d" % i, [128, D], F32) for i in range(2)]
        for t in range(NTILE):
            a = xt[t % 2]
            if t < 16:
                b = pt[t % 2]
                kb.dma(kb.sp, a, G["x"][t * 128:(t + 1) * 128, :])
                kb.dma(kb.sp, b, G["pos"][t * 128:(t + 1) * 128, :])
                kb.tt(a[:, :], a[:, :], b[:, :], ALU.add)
            else:
                kb.dma(kb.sp, a, G["ctx"][(t - 16) * 128:(t - 15) * 128, :])
            kb.dma(kb.sp, C.h[t], a)
        kb.barrier()


def phase_mod(kb, C, G, l):
    with ExitStack() as st:
        cl = kb.sbuf(st, "cl", [128, 8], F32)
        cc = kb.sbuf(st, "cc", [128, 8], F32)
        sc = kb.sbuf(st, "sc", [128, 8, 2], F32)
        bm = kb.sbuf(st, "bm", [128, 48], F32)
        wm = [kb.sbuf(st, "wm%d" % i, [128, 8, 768], F32) for i in range(2)]
        ps = kb.psum(st, "psmod", [128, 48, 2], F32)
        kb.dma(kb.sp, cl, G["cT"])
        kb.dma(kb.sp, cc, G["cctxT"])
        kb.dma(kb.sp, bm, G["b_modT"][l])
        kb.actf(sc[:, :, 0], cl[:, :], AF.Silu)
        kb.actf(sc[:, :, 1], cc[:, :], AF.Silu)
        wsrc = G["w_mod"][l].re("(k p) n -> p k n", p=128)
        for ng in range(8):
            w = wm[ng % 2]
            kb.dma(kb.sp, w, wsrc[:, :, ng * 768:(ng + 1) * 768])
            for j in range(6):
                ch = ng * 6 + j
                for k in range(8):
                    kb.mm(ps[:, ch, :], w[:, k, j * 128:(j + 1) * 128], sc[:, k, :], start=(k == 0), stop=(k == 7))
        kb.tt(C.modT[:, :, :], ps[:, :, :], View(bm, bm.ap.unsqueeze(2).broadcast_to([128, 48, 2])), ALU.add)
        kb.ts(C.onep[:, 0:8, :], C.modT[:, 8:16, :], 1.0, None, ALU.add)
        kb.ts(C.onep[:, 8:16, :], C.modT[:, 32:40, :], 1.0, None, ALU.add)
        kb.barrier()


def bcast_from_modT(kb, dg, ps, C, idx, seg, out):
    for c in range(8):
        kb.ts(dg[:, :], C.ident[:, :], C.modT[:, idx + c, seg:seg + 1], None, ALU.mult)
        kb.mm(ps[:, c * 128:(c + 1) * 128], C.ones[:, :], dg[:, :])
    kb.copy(out, ps[:, :])


def lnT_alloc(kb, st, C, G, l, which, router=None, npst=2):
    L = Ctx()
    L.sh_idx = 0 if which == 0 else 24
    L.op_idx = 0 if which == 0 else 8
    L.ht = [kb.sbuf(st, "ht%d" % i, [128, D], F32) for i in range(2)]
    L.xc = [kb.sbuf(st, "xc%d" % i, [128, D], F32) for i in range(2)]
    L.junk2 = [kb.sbuf(st, "junk%d" % i, [128, D], F32) for i in range(2)]
    L.sm = [kb.sbuf(st, "sm%d" % i, [128, 4], F32) for i in range(2)]
    L.pst = [kb.psum(st, "pst%d" % i, [128, 8, 128], F32) for i in range(npst)]
    if router is not None:
        L.x32 = [kb.sbuf(st, "x32%d" % i, [128, 8, 128], F32) for i in range(2)]
        L.rw = kb.sbuf(st, "rw", [128, 8, 20], F32)
        L.rb = kb.sbuf(st, "rb", [1, 20], F32)
        kb.dma(kb.sp, L.rw, G["rw"][l].re("(k p) n -> p k n", p=128))
        kb.dma(kb.sp, L.rb, G["rb"][l:l + 1, :])
        L.plg = [kb.psum(st, "plg%d" % i, [128, 32], F32) for i in range(2)]
        L.R2 = [{k: kb.sbuf(st, "r%d_" % i + k, [128, n], F32) for k, n in
                 [("lg", 20), ("oh", 4), ("ex", 4), ("t16", 16), ("es", 4), ("k1", 4), ("e2", 4), ("k2", 4),
                  ("t4", 4), ("cs", 4), ("s", 12)]} for i in range(2)]
    return L


def lnT_tile_g(kb, C, L, t, xT, router=None):
    seg = seg_of_tile(t)
    h, x, s_, p = L.ht[t % 2], L.xc[t % 2], L.sm[t % 2], L.pst[t % len(L.pst)]
    junk = L.junk2[t % 2]
    kb.dma(kb.sp, h, C.h[t])
    yield
    yield from ln_stats_g(kb, h[:, :], x[:, :], junk[:, :], s_)
    kb.actf(x[:, :], x[:, :], AF.Identity, scale=s_[:, 3:4])
    yield
    for c in range(8):
        kb.mm(p[:, c, :], x[:, c * 128:(c + 1) * 128], C.ident[:, :])
    yield
    for c in range(8):
        dst = L.x32[t % 2][:, c, :] if router is not None else xT[:, c, t * 128:(t + 1) * 128]
        kb.actf(dst, p[:, c, :], AF.Identity, bias=C.modT[:, L.sh_idx + c, seg:seg + 1],
                scale=C.onep[:, L.op_idx + c, seg:seg + 1])
        if c % 4 == 3:
            yield
    if router is not None:
        xx = L.x32[t % 2]
        kb.copy(xT[:, :, t * 128:(t + 1) * 128], xx[:, :, :])
        lg = L.plg[t % 2]
        for c in range(8):
            kb.mm(lg[:, 0:20], xx[:, c, :], L.rw[:, c, :], start=(c == 0), stop=False)
        kb.mm(lg[:, 0:20], C.ones[0:1, :], L.rb[0:1, :], start=False, stop=True)
        yield
        yield from route_g(kb, L.R2[t % 2], lg, router[:, t, :])


def lnT_tile(kb, C, L, t, xT, router=None):
    for _ in lnT_tile_g(kb, C, L, t, xT, router):
        pass


def lnT_tiles(kb, C, L, tiles, xT, router=None):
    tiles = list(tiles)
    for i in range(0, len(tiles), 2):
        lockstep([lnT_tile_g(kb, C, L, t, xT, router) for t in tiles[i:i + 2]])


def phase_lnT(kb, C, G, l, which, xT, router=None, ntile=NTILE):
    with ExitStack() as st:
        L = lnT_alloc(kb, st, C, G, l, which, router)
        lnT_tiles(kb, C, L, range(ntile), xT, router)
        kb.barrier()


def route_g(kb, R, lg, comb):
    s = R["s"]
    gmax, ngmax, gs, gp, m1, m2, dd, ex2, w1, w2 = (s[:, i:i + 1] for i in range(10))
    L = R["lg"]
    kb.copy(L[:, :], lg[:, 0:20])
    yield
    kb.reduce(gmax, L[:, 0:4], ALU.max)
    yield
    kb.ts(R["oh"][:, :], L[:, 0:4], gmax, None, ALU.is_equal)
    yield
    kb.ts(ngmax, gmax, -1.0, None, ALU.mult)
    yield
    kb.actf(R["ex"][:, :], L[:, 0:4], AF.Exp, bias=ngmax, accum_out=gs)
    yield
    kb.recip(gp, gs)
    yield
    oh = R["oh"]
    kb.tt(R["t16"][:, :].re("p (g e) -> p g e", g=4), L[:, 4:20].re("p (g e) -> p g e", g=4),
          View(oh, oh.ap.unsqueeze(2).broadcast_to([128, 4, 4])), ALU.mult)
    yield
    kb.reduce(R["es"][:, :], R["t16"][:, :].re("p (g e) -> p e g", g=4), ALU.add)
    yield
    kb.reduce(m1, R["es"][:, :], ALU.max)
    yield
    kb.ts(R["k1"][:, :], R["es"][:, :], m1, None, ALU.is_equal)
    yield
    kb.stt(R["e2"][:, :], R["k1"][:, :], -1e30, R["es"][:, :], ALU.mult, ALU.add)
    yield
    kb.reduce(m2, R["e2"][:, :], ALU.max)
    yield
    kb.ts(R["k2"][:, :], R["e2"][:, :], m2, None, ALU.is_equal)
    yield
    kb.tt(dd, m2, m1, ALU.subtract)
    yield
    kb.actf(ex2, dd, AF.Exp)
    yield
    kb.ts(w1, ex2, 1.0, None, ALU.add)
    yield
    kb.recip(w1, w1)
    yield
    kb.tt(w2, ex2, w1, ALU.mult)
    yield
    kb.tt(w1, w1, gp, ALU.mult)
    yield
    kb.tt(w2, w2, gp, ALU.mult)
    yield
    kb.ts(R["t4"][:, :], R["k1"][:, :], w1, None, ALU.mult)
    yield
    kb.stt(R["cs"][:, :], R["k2"][:, :], w2, R["t4"][:, :], ALU.mult, ALU.add)
    yield
    cs = R["cs"]
    kb.tt(comb.re("p (g e) -> p g e", g=4), View(oh, oh.ap.unsqueeze(2).broadcast_to([128, 4, 4])),
          View(cs, cs.ap.unsqueeze(1).broadcast_to([128, 4, 4])), ALU.mult)
    yield


def phase_inproj(kb, C, G, l, xT):
    with ExitStack() as st:
        wt = [kb.sbuf(st, "wi%d" % i, [128, 8, 128], BF16) for i in range(3)]
        stg = [kb.sbuf(st, "stg%d" % i, [128, NT], F32) for i in range(2)]
        bi = kb.sbuf(st, "bi", [128, NCH], F32)
        pp = [kb.psum(st, "pp%d" % i, [128, 512], F32) for i in range(4)]
        L = lnT_alloc(kb, st, C, G, l, 0, None)
        kb.dma(kb.sp, bi, G["b_inT"][l])
        n = 0
        for ch in range(NCH):
            w = wt[ch % 3]
            kb.dma(kb.pool, w, G["w_inR"][l, ch])
            rows = 128 if ch < 68 else 16
            sg = stg[ch % 2]
            for (t0, tl) in TBLK:
                if ch == 0:
                    lnT_tiles(kb, C, L, range(t0 // 128, (t0 + tl) // 128), xT)
                p = pp[n % 4]
                n += 1
                for k in range(8):
                    kb.mm(p[0:rows, 0:tl], w[:, k, 0:rows], xT[:, k, t0:t0 + tl], start=(k == 0), stop=(k == 7))
                kb.actf(sg[0:rows, t0:t0 + tl], p[0:rows, 0:tl], AF.Identity, bias=bi[0:rows, ch:ch + 1])
            kb.dma(kb.sp, C.pT[ch][0:rows, :], sg[0:rows, :])
        kb.barrier()


def group_ln_fm(kb, C, st, x, out, n, gcol, bcol, tmp, psA, psB):
    sq, r = tmp
    kb.actf(sq[:, 0:n], x, AF.Square)
    kb.mm(psA[:, 0:n], C.onesm[:, :], x)
    kb.mm(psB[:, 0:n], C.onesm[:, :], sq[:, 0:n])
    kb.actf(sq[:, 0:n], psA[:, 0:n], AF.Square)
    kb.tt(r[:, 0:n], psB[:, 0:n], sq[:, 0:n], ALU.subtract)
    kb.ts(r[:, 0:n], r[:, 0:n], EPS, None, ALU.add)
    kb.actf(r[:, 0:n], r[:, 0:n], AF.Sqrt)
    kb.recip(r[:, 0:n], r[:, 0:n])
    kb.tt(sq[:, 0:n], x, psA[:, 0:n], ALU.subtract)
    kb.tt(sq[:, 0:n], sq[:, 0:n], r[:, 0:n], ALU.mult)
    kb.ts(out, sq[:, 0:n], gcol, bcol, ALU.mult, ALU.add)


def group_ln_fm_g(kb, C, st, x, out, n, gcol, bcol, tmp, psA, psB):
    sq, r = tmp
    kb.actf(sq[:, 0:n], x, AF.Square)
    yield
    kb.mm(psA[:, 0:n], C.onesm[:, :], x)
    yield
    kb.mm(psB[:, 0:n], C.onesm[:, :], sq[:, 0:n])
    yield
    kb.actf(sq[:, 0:n], psA[:, 0:n], AF.Square)
    yield
    kb.tt(r[:, 0:n], psB[:, 0:n], sq[:, 0:n], ALU.subtract)
    yield
    kb.ts(r[:, 0:n], r[:, 0:n], EPS, None, ALU.add)
    yield
    kb.actf(r[:, 0:n], r[:, 0:n], AF.Sqrt)
    yield
    kb.recip(r[:, 0:n], r[:, 0:n])
    yield
    kb.tt(sq[:, 0:n], x, psA[:, 0:n], ALU.subtract)
    yield
    kb.tt(sq[:, 0:n], sq[:, 0:n], r[:, 0:n], ALU.mult)
    yield
    kb.ts(out, sq[:, 0:n], gcol, bcol, ALU.mult, ALU.add)
    yield


def phase_conformer(kb, C, G, l):
    with ExitStack() as st:
        a = [kb.sbuf(st, "ca%d" % i, [128, NT], F32) for i in range(2)]
        g = [kb.sbuf(st, "cg%d" % i, [128, NT], F32) for i in range(2)]
        pad = [kb.sbuf(st, "cpad%d" % i, [128, NT + 60], BF16) for i in range(2)]
        dg = [kb.sbuf(st, "cdg%d" % i, [128, 31, 128], BF16) for i in range(2)]
        ob = kb.sbuf(st, "cob", [128, NT], BF16)
        tmp = [kb.sbuf(st, "ctmp%d" % i, [128, 512], F32) for i in range(2)]
        lnx = [kb.sbuf(st, "clnx%d" % i, [128, 512], F32) for i in range(2)]
        cw = kb.sbuf(st, "cw", [128, 4, 31], F32)
        cv = kb.sbuf(st, "cv", [128, 3, 4], F32)
        psA = kb.psum(st, "cpsA", [128, 512], F32)
        psB = kb.psum(st, "cpsB", [128, 512], F32)
        pc = [kb.psum(st, "cpc%d" % i, [128, 512], F32) for i in range(2)]
        kb.dma(kb.sp, cw, G["conf_wT"][l])
        kb.dma(kb.sp, cv, G["conf_vT"][l])
        for i in range(2):
            kb.memset(pad[i][:, :], 0.0)
        poff = [15, 2048 + 45]
        nb = 0
        for ch in range(4):
            a_, g_, pad_, dg_ = a[ch % 2], g[ch % 2], pad[ch % 2], dg[ch % 2]
            kb.dma(kb.sp, a_, C.pT[O_CA // 128 + ch])
            kb.dma(kb.sp, g_, C.pT[O_CG // 128 + ch])
            kb.actf(g_[:, :], g_[:, :], AF.Sigmoid)
            for si, (t0, tl) in enumerate(SEGS):
                kb.tt(pad_[:, poff[si]:poff[si] + tl], a_[:, t0:t0 + tl], g_[:, t0:t0 + tl], ALU.mult)
            for k in range(31):
                kb.ts(dg_[:, k, :], C.identb[:, :], cw[:, ch, k:k + 1], None, ALU.mult)
            for (t0, tl) in TBLK:
                si = 0 if t0 < 2048 else 1
                off = poff[si] - 15 + (t0 - SEGS[si][0])
                p = pc[nb % 2]
                x = lnx[nb % 2]
                nb += 1
                for k in range(31):
                    kb.mm(p[:, 0:tl], dg_[:, k, :], pad_[:, off + k:off + k + tl], start=(k == 0), stop=(k == 30))
                kb.actf(x[:, 0:tl], p[:, 0:tl], AF.Identity, bias=cv[:, 0, ch:ch + 1])
                group_ln_fm(kb, C, st, x[:, 0:tl], x[:, 0:tl], tl, cv[:, 1, ch:ch + 1], cv[:, 2, ch:ch + 1], tmp, psA, psB)
                kb.actf(ob[:, t0:t0 + tl], x[:, 0:tl], AF.Silu)
            kb.dma(kb.sp, C.brT[1][ch], ob)
        kb.barrier()


def phase_conformer_g(kb, C, G, l, st, npc=2):
    if True:
        a = [kb.sbuf(st, "ca%d" % i, [128, NT], F32) for i in range(2)]
        g = [kb.sbuf(st, "cg%d" % i, [128, NT], F32) for i in range(2)]
        pad = [kb.sbuf(st, "cpad%d" % i, [128, NT + 60], BF16) for i in range(2)]
        dg = [kb.sbuf(st, "cdg%d" % i, [128, 31, 128], BF16) for i in range(2)]
        ob = kb.sbuf(st, "cob", [128, NT], BF16)
        tmp = [kb.sbuf(st, "ctmp%d" % i, [128, 512], F32) for i in range(2)]
        lnx = [kb.sbuf(st, "clnx%d" % i, [128, 512], F32) for i in range(2)]
        cw = kb.sbuf(st, "cw", [128, 4, 31], F32)
        cv = kb.sbuf(st, "cv", [128, 3, 4], F32)
        psA = kb.psum(st, "cpsA", [128, 512], F32)
        psB = kb.psum(st, "cpsB", [128, 512], F32)
        pc = [kb.psum(st, "cpc%d" % i, [128, 512], F32) for i in range(npc)]
        kb.dma(kb.sp, cw, G["conf_wT"][l])
        yield
        kb.dma(kb.sp, cv, G["conf_vT"][l])
        yield
        for i in range(2):
            kb.memset(pad[i][:, :], 0.0)
            yield
        poff = [15, 2048 + 45]
        nb = 0
        for ch in range(4):
            a_, g_, pad_, dg_ = a[ch % 2], g[ch % 2], pad[ch % 2], dg[ch % 2]
            kb.dma(kb.sp, a_, C.pT[O_CA // 128 + ch])
            yield
            kb.dma(kb.sp, g_, C.pT[O_CG // 128 + ch])
            yield
            kb.actf(g_[:, :], g_[:, :], AF.Sigmoid)
            yield
            for si, (t0, tl) in enumerate(SEGS):
                kb.tt(pad_[:, poff[si]:poff[si] + tl], a_[:, t0:t0 + tl], g_[:, t0:t0 + tl], ALU.mult)
                yield
            for k in range(31):
                kb.ts(dg_[:, k, :], C.identb[:, :], cw[:, ch, k:k + 1], None, ALU.mult)
                yield
            for (t0, tl) in TBLK:
                si = 0 if t0 < 2048 else 1
                off = poff[si] - 15 + (t0 - SEGS[si][0])
                p = pc[nb % npc]
                x = lnx[nb % 2]
                nb += 1
                for k in range(31):
                    kb.mm(p[:, 0:tl], dg_[:, k, :], pad_[:, off + k:off + k + tl], start=(k == 0), stop=(k == 30))
                    yield
                kb.actf(x[:, 0:tl], p[:, 0:tl], AF.Identity, bias=cv[:, 0, ch:ch + 1])
                yield
                yield from group_ln_fm_g(kb, C, st, x[:, 0:tl], x[:, 0:tl], tl, cv[:, 1, ch:ch + 1], cv[:, 2, ch:ch + 1], tmp, psA, psB)
                kb.actf(ob[:, t0:t0 + tl], x[:, 0:tl], AF.Silu)
                yield
            kb.dma(kb.sp, C.brT[1][ch], ob)
            yield


def gelu_tanh(kb, x, out, t1, t2, n):
    kb.actf(t1, x, AF.Square)
    kb.ts(t1, t1, 0.044715, 1.0, ALU.mult, ALU.add)
    kb.tt(t1, t1, x, ALU.mult)
    kb.actf(t2, t1, AF.Sigmoid, scale=GELU_C)
    kb.tt(out, x, t2, ALU.mult)


def gelu_tanh_g(kb, x, out, t1, t2, n):
    kb.actf(t1, x, AF.Square)
    yield
    kb.ts(t1, t1, 0.044715, 1.0, ALU.mult, ALU.add)
    yield
    kb.tt(t1, t1, x, ALU.mult)
    yield
    kb.actf(t2, t1, AF.Sigmoid, scale=GELU_C)
    yield
    kb.tt(out, x, t2, ALU.mult)
    yield


def phase_sgu(kb, C, G, l):
    with ExitStack() as st:
        u = kb.sbuf(st, "su", [128, NT], F32)
        v = kb.sbuf(st, "sv", [128, NT], F32)
        t1 = kb.sbuf(st, "st1", [128, NT], F32)
        t2 = kb.sbuf(st, "st2", [128, NT], F32)
        ob = kb.sbuf(st, "sob", [128, NT], BF16)
        tmp = [kb.sbuf(st, "stmp%d" % i, [128, 512], F32) for i in range(2)]
        vt = [kb.sbuf(st, "svt%d" % i, [128, 4, 128], BF16) for i in range(2)]
        sv = kb.sbuf(st, "svv", [128, 2, 4], F32)
        ws = kb.sbuf(st, "sws", [128, 4, 128], BF16)
        bsb = kb.sbuf(st, "bsb", [128, 4, 128], F32)
        psA = kb.psum(st, "spsA", [128, 512], F32)
        psB = kb.psum(st, "spsB", [128, 512], F32)
        pst = [kb.psum(st, "spst%d" % i, [128, 512], F32) for i in range(2)]
        pss = [kb.psum(st, "spss%d" % i, [128, 512], F32) for i in range(2)]
        kb.dma(kb.sp, sv, G["sgu_vT"][l])
        kb.dma(kb.pool, ws, G["sgu_wsT"][l].re("g q p -> q g p"))
        kb.dma(kb.sp, bsb[:, :, :].re("p g q -> p (g q)"),
               View(G["sgu_bs"], G["sgu_bs"].ap[l].rearrange("g p -> (g p)").partition_broadcast(128)))
        for g in range(4):
            kb.dma(kb.sp, u, C.pT[O_SU // 128 + g])
            kb.dma(kb.sp, v, C.pT[O_SV // 128 + g])
            gelu_tanh(kb, u[:, :], u[:, :], t1[:, :], t2[:, :], NT)
            gelu_tanh(kb, v[:, :], v[:, :], t1[:, :], t2[:, :], NT)
            for (t0, tl) in TBLK:
                group_ln_fm(kb, C, st, v[:, t0:t0 + tl], v[:, t0:t0 + tl], tl, sv[:, 0, g:g + 1], sv[:, 1, g:g + 1], tmp, psA, psB)
            for bi, (t0, tl) in enumerate(TBLK):
                nt = tl // 128
                pt_, ps_, vv = pst[bi % 2], pss[bi % 2], vt[bi % 2]
                for j in range(nt):
                    kb.mm(pt_[:, j * 128:(j + 1) * 128], v[:, t0 + j * 128:t0 + (j + 1) * 128], C.ident[:, :])
                kb.copy(vv[:, 0:nt, :].re("p a b -> p (a b)"), pt_[:, 0:tl], eng=kb.act)
                for j in range(nt):
                    kb.mm(ps_[:, j * 128:(j + 1) * 128], vv[:, j, :], ws[:, g, :])
                kb.tt(t1[:, t0:t0 + tl].re("p (a b) -> p a b", a=nt), ps_[:, 0:tl].re("p (a b) -> p a b", a=nt),
                      View(bsb, bsb.ap[:, g, :].unsqueeze(1).broadcast_to([128, nt, 128])), ALU.add)
                kb.tt(ob[:, t0:t0 + tl], t1[:, t0:t0 + tl], u[:, t0:t0 + tl], ALU.mult)
            kb.dma(kb.sp, C.brT[3][g], ob)
        kb.barrier()


def phase_sgu_g(kb, C, G, l, st):
    if True:
        u = kb.sbuf(st, "su", [128, NT], F32)
        v = kb.sbuf(st, "sv", [128, NT], F32)
        t1 = kb.sbuf(st, "st1", [128, NT], F32)
        t2 = kb.sbuf(st, "st2", [128, NT], F32)
        ob = kb.sbuf(st, "sob", [128, NT], BF16)
        tmp = [kb.sbuf(st, "stmp%d" % i, [128, 512], F32) for i in range(2)]
        vt = [kb.sbuf(st, "svt%d" % i, [128, 4, 128], BF16) for i in range(2)]
        sv = kb.sbuf(st, "svv", [128, 2, 4], F32)
        ws = kb.sbuf(st, "sws", [128, 4, 128], BF16)
        bsb = kb.sbuf(st, "bsb", [128, 4, 128], F32)
        psA = kb.psum(st, "spsA", [128, 512], F32)
        psB = kb.psum(st, "spsB", [128, 512], F32)
        pst = [kb.psum(st, "spst%d" % i, [128, 512], F32) for i in range(1)] * 2
        pss = [kb.psum(st, "spss%d" % i, [128, 512], F32) for i in range(1)] * 2
        kb.dma(kb.sp, sv, G["sgu_vT"][l])
        yield
        kb.dma(kb.pool, ws, G["sgu_wsT"][l].re("g q p -> q g p"))
        yield
        kb.dma(kb.sp, bsb[:, :, :].re("p g q -> p (g q)"),
               View(G["sgu_bs"], G["sgu_bs"].ap[l].rearrange("g p -> (g p)").partition_broadcast(128)))
        yield
        for g in range(4):
            kb.dma(kb.sp, u, C.pT[O_SU // 128 + g])
            yield
            kb.dma(kb.sp, v, C.pT[O_SV // 128 + g])
            yield
            yield from gelu_tanh_g(kb, u[:, :], u[:, :], t1[:, :], t2[:, :], NT)
            yield from gelu_tanh_g(kb, v[:, :], v[:, :], t1[:, :], t2[:, :], NT)
            for (t0, tl) in TBLK:
                yield from group_ln_fm_g(kb, C, st, v[:, t0:t0 + tl], v[:, t0:t0 + tl], tl, sv[:, 0, g:g + 1], sv[:, 1, g:g + 1], tmp, psA, psB)
            for bi, (t0, tl) in enumerate(TBLK):
                nt = tl // 128
                pt_, ps_, vv = pst[bi % 2], pss[bi % 2], vt[bi % 2]
                for j in range(nt):
                    kb.mm(pt_[:, j * 128:(j + 1) * 128], v[:, t0 + j * 128:t0 + (j + 1) * 128], C.ident[:, :])
                    yield
                kb.copy(vv[:, 0:nt, :].re("p a b -> p (a b)"), pt_[:, 0:tl], eng=kb.act)
                yield
                for j in range(nt):
                    kb.mm(ps_[:, j * 128:(j + 1) * 128], vv[:, j, :], ws[:, g, :])
                    yield
                kb.tt(t1[:, t0:t0 + tl].re("p (a b) -> p a b", a=nt), ps_[:, 0:tl].re("p (a b) -> p a b", a=nt),
                      View(bsb, bsb.ap[:, g, :].unsqueeze(1).broadcast_to([128, nt, 128])), ALU.add)
                yield
                kb.tt(ob[:, t0:t0 + tl], t1[:, t0:t0 + tl], u[:, t0:t0 + tl], ALU.mult)
                yield
            kb.dma(kb.sp, C.brT[3][g], ob)
            yield


def phase_fft(kb, C, G, l):
    with ExitStack() as st:
        hT = kb.sbuf(st, "fh", [128, 4, NT], BF16)
        cs = kb.sbuf(st, "fcs", [128, 256], BF16)
        AB = kb.sbuf(st, "fab", [128, NTILE, 4, 256], BF16)
        tab = [kb.sbuf(st, "ftab%d" % i, [128, 2, 16, 512], BF16) for i in range(2)]
        ob = kb.sbuf(st, "fob", [128, 4, NT], BF16)
        pa = [kb.psum(st, "fpa%d" % i, [128, 256], F32) for i in range(2)]
        py = [kb.psum(st, "fpy%d" % i, [128, 512], F32) for i in range(2)]
        kb.dma(kb.pool, cs, G["dft_c"])
        for g in range(4):
            kb.dma(kb.pool, hT[:, g, :], C.pT[O_F // 128 + g])
        n = 0
        for t in range(NTILE):
            for g in range(4):
                p = pa[n % 2]
                kb.mm(p[:, :], hT[:, g, t * 128:(t + 1) * 128], cs[:, :])
                kb.copy(AB[:, t, g, :], p[:, :], eng=kb.act if n % 2 == 0 else kb.dve)
                n += 1
        n = 0
        for kbk in range(4):
            tb = tab[kbk % 2]
            for cs_i in range(2):
                kb.dma(kb.pool, tb[:, cs_i, :, :], G["dft_lat"][cs_i].re("(t p) k -> p t k", p=128)[:, :, kbk * 512:(kbk + 1) * 512])
            for g in range(4):
                p = py[n % 2]
                for t in range(16):
                    kb.mm(p[:, :], AB[:, t, g, 0:128], tb[:, 0, t, :], start=(t == 0), stop=False)
                    kb.mm(p[:, :], AB[:, t, g, 128:256], tb[:, 1, t, :], start=False, stop=(t == 15))
                kb.copy(ob[:, g, kbk * 512:(kbk + 1) * 512], p[:, :], eng=kb.act if n % 2 == 0 else kb.dve)
                n += 1
        tb = tab[0]
        for cs_i in range(2):
            kb.dma(kb.pool, tb[:, cs_i, 0:2, 0:256], G["dft_ctx"][cs_i].re("(t p) k -> p t k", p=128))
        for g in range(4):
            p = py[n % 2]
            for t in range(2):
                kb.mm(p[:, 0:256], AB[:, 16 + t, g, 0:128], tb[:, 0, t, 0:256], start=(t == 0), stop=False)
                kb.mm(p[:, 0:256], AB[:, 16 + t, g, 128:256], tb[:, 1, t, 0:256], start=False, stop=(t == 1))
            kb.copy(ob[:, g, 2048:2304], p[:, 0:256], eng=kb.act if n % 2 == 0 else kb.dve)
            n += 1
        for g in range(4):
            kb.dma(kb.sp, C.brT[2][g], ob[:, g, :])
        kb.barrier()


def phase_merge(kb, C, G, l, last=False):
    blks = TBLK[:4] if last else TBLK
    ntl = 16 if last else NTILE
    with ExitStack() as st0:
      wo = kb.sbuf(st0, "mwo", [128, 8, D], BF16)
      mT = kb.sbuf(st0, "mT", [128, 8, NT], BF16)
      with ExitStack() as st:
        br = kb.sbuf(st, "mbr", [128, 16, NT], BF16)
        wb = kb.sbuf(st, "mwb", [128, 16, D], BF16)
        gt = [kb.sbuf(st, "mgt%d" % i, [128, NT], F32) for i in range(2)]
        macc = kb.sbuf(st, "macc", [128, NT], F32)
        mtmp = [kb.sbuf(st, "mtmp%d" % i, [128, 512], F32) for i in range(2)]
        pp = [kb.psum(st, "mpp%d" % i, [128, 512], F32) for i in range(2)]
        for i in range(4):
            for kc in range(4):
                kb.dma(kb.sp, br[:, i * 4 + kc, :], C.brT[i][kc])
            kb.dma(kb.pool, wb[:, i * 4:(i + 1) * 4, :], G["w_branch"][l, i].re("(k p) d -> p k d", p=128))
        kb.dma(kb.pool, wo, G["w_out"][l].re("(k p) d -> p k d", p=128))
        def branch_g(dc, i):
            g = gt[i % 2]
            p = pp[i % 2]
            tm = mtmp[i % 2]
            kb.dma(kb.sp, g, C.pT[O_G // 128 + i * 8 + dc])
            yield
            kb.actf(g[:, :], g[:, :], AF.Sigmoid)
            yield
            for bi, (t0, tl) in enumerate(blks):
                for kc in range(4):
                    kb.mm(p[:, 0:tl], wb[:, i * 4 + kc, dc * 128:(dc + 1) * 128], br[:, i * 4 + kc, t0:t0 + tl],
                          start=(kc == 0), stop=(kc == 3))
                yield
                if i == 0:
                    kb.tt(macc[:, t0:t0 + tl], p[:, 0:tl], g[:, t0:t0 + tl], ALU.mult)
                    yield
                    yield
                else:
                    kb.tt(tm[:, 0:tl], p[:, 0:tl], g[:, t0:t0 + tl], ALU.mult)
                    yield
                    kb.tt(macc[:, t0:t0 + tl], macc[:, t0:t0 + tl], tm[:, 0:tl], ALU.add)
                    yield
        for dc in range(8):
            for i0 in (0, 2):
                lockstep([branch_g(dc, i0), branch_g(dc, i0 + 1)])
            kb.copy(mT[:, dc, :], macc[:, :], eng=kb.act)
        kb.barrier()
      if True:
        with ExitStack() as st2:
            g1 = [kb.sbuf(st2, "g1_%d" % i, [128, D], F32) for i in range(2)]
            bo = kb.sbuf(st2, "bo", [128, D], F32)
            lg = kb.sbuf(st2, "lg1", [128, D], F32)
            lb = kb.sbuf(st2, "lb1", [128, D], F32)
            dg = kb.sbuf(st2, "dg", [128, 128], F32)
            psbc = kb.psum(st2, "psbc", [128, 1024], F32)
            for seg in range(2):
                bcast_from_modT(kb, dg, psbc, C, 16, seg, g1[seg][:, :])
            for nm, dst in (("b_out", bo), ("ln1_g", lg), ("ln1_b", lb)):
                kb.dma(kb.sp, dst, View(G[nm], G[nm].ap[l].partition_broadcast(128)))
            residual_ln(kb, C, st2, lambda t, dh, p: [kb.mm(p[:, :], mT[:, k, t * 128:(t + 1) * 128], wo[:, k, dh * 512:(dh + 1) * 512],
                                                          start=(k == 0), stop=(k == 7)) for k in range(8)],
                        g1, bo, lg, lb, None, ntile=ntl)
            kb.barrier()


def residual_ln(kb, C, st, emit_mm, gbc, bias_bc, lng, lnb, acc, out_final=None, ntile=NTILE):
    ht = [kb.sbuf(st, "rh%d" % i, [128, D], F32) for i in range(2)]
    yt = [kb.sbuf(st, "ry%d" % i, [128, D], F32) for i in range(2)]
    junk = [kb.sbuf(st, "rjunk%d" % i, [128, D], F32) for i in range(2)]
    sm = [kb.sbuf(st, "rsm%d" % i, [128, 4], F32) for i in range(2)]
    pp = [kb.psum(st, "rpp%d" % i, [128, 512], F32) for i in range(4)] if emit_mm is not None else None

    def tile_g(t):
        seg = seg_of_tile(t)
        h, y, s_ = ht[t % 2], yt[t % 2], sm[t % 2]
        kb.dma(kb.sp, h, C.h[t])
        yield
        if emit_mm is not None:
            for dh in range(2):
                p = pp[(t * 2 + dh) % 4]
                emit_mm(t, dh, p)
                kb.tt(y[:, dh * 512:(dh + 1) * 512], p[:, :], bias_bc[:, dh * 512:(dh + 1) * 512], ALU.add)
                yield
            kb.tt(y[:, :], y[:, :], gbc[seg][:, :], ALU.mult)
        else:
            kb.tt(y[:, :], acc[t][:, :], gbc[seg][:, :], ALU.mult)
        yield
        kb.stt(h[:, :], h[:, :], ALPHA, y[:, :], ALU.mult, ALU.add)
        yield
        yield from ln_stats_g(kb, h[:, :], y[:, :], junk[t % 2][:, :], s_)
        kb.actf(y[:, :], y[:, :], AF.Identity, scale=s_[:, 3:4])
        yield
        kb.tt(y[:, :], y[:, :], lng[:, :], ALU.mult)
        yield
        kb.tt(y[:, :], y[:, :], lnb[:, :], ALU.add)
        yield
        if out_final is not None:
            if t < 16:
                kb.dma(kb.sp, out_final[t * 128:(t + 1) * 128, :], y)
        else:
            kb.dma(kb.sp, C.h[t], y)
    for t0 in range(0, ntile, 2):
        lockstep([tile_g(t) for t in range(t0, min(t0 + 2, ntile))])


def phase_moe(kb, C, G, l, last):
    with ExitStack() as st:
        xT = kb.sbuf(st, "xT2", [128, 8, NT], BF16)
        comb = kb.sbuf(st, "comb", [128, NTILE, 16], F32)
        ntl = 16 if last else NTILE
        blks = TBLK[:4] if last else TBLK
        phase_lnT(kb, C, G, l, 1, xT, router=comb, ntile=ntl)
        acc = [kb.sbuf(st, "acc%d" % t, [128, D], F32) for t in range(NTILE)]
        with ExitStack() as st1:
            wg = [kb.sbuf(st1, "wg%d" % i, [128, 8, 512], BF16) for i in range(2)]
            wu = [kb.sbuf(st1, "wu%d" % i, [128, 8, 512], BF16) for i in range(2)]
            wd = [kb.sbuf(st1, "wd%d" % i, [128, 4, D], BF16) for i in range(2)]
            hid = [kb.sbuf(st1, "hid%d" % i, [128, 4, 512], BF16) for i in range(2)]
            sg = [kb.sbuf(st1, "sg%d" % i, [128, 512], F32) for i in range(2)]
            pg = [kb.psum(st1, "pg%d" % i, [128, 512], F32) for i in range(2)]
            pu = [kb.psum(st1, "pu%d" % i, [128, 512], F32) for i in range(2)]
            po = [kb.psum(st1, "po%d" % i, [128, 512], F32) for i in range(4)]
            n = 0
            m = 0
            for e in range(16):
                g_, u_, d_ = wg[e % 2], wu[e % 2], wd[e % 2]
                kb.dma(kb.pool, g_, G["w_gate"][l, e].re("(k p) h -> p k h", p=128))
                kb.dma(kb.pool, u_, G["w_up"][l, e].re("(k p) h -> p k h", p=128))
                kb.dma(kb.pool, d_, G["w_down"][l, e].re("(k p) d -> p k d", p=128))
                for bi, (t0, tl) in enumerate(blks):
                    hd = hid[bi % 2]
                    for hc in range(4):
                        a, b, s_ = pg[n % 2], pu[n % 2], sg[n % 2]
                        n += 1
                        for k in range(8):
                            kb.mm(a[:, 0:tl], g_[:, k, hc * 128:(hc + 1) * 128], xT[:, k, t0:t0 + tl], start=(k == 0), stop=(k == 7))
                        for k in range(8):
                            kb.mm(b[:, 0:tl], u_[:, k, hc * 128:(hc + 1) * 128], xT[:, k, t0:t0 + tl], start=(k == 0), stop=(k == 7))
                        kb.actf(s_[:, 0:tl], a[:, 0:tl], AF.Silu)
                        kb.tt(hd[:, hc, 0:tl], s_[:, 0:tl], b[:, 0:tl], ALU.mult)
                    for tt_ in range(tl // 128):
                        t = t0 // 128 + tt_
                        for dh in range(2):
                            p = po[m % 4]
                            m += 1
                            for hc in range(4):
                                kb.mm(p[:, :], hd[:, hc, tt_ * 128:(tt_ + 1) * 128], d_[:, hc, dh * 512:(dh + 1) * 512],
                                      start=(hc == 0), stop=(hc == 3))
                            dst = acc[t][:, dh * 512:(dh + 1) * 512]
                            if e == 0:
                                kb.ts(dst, p[:, :], comb[:, t, e:e + 1], None, ALU.mult)
                            else:
                                kb.stt(dst, p[:, :], comb[:, t, e:e + 1], dst, ALU.mult, ALU.add)
            kb.barrier()
        with ExitStack() as st2:
            g2 = [kb.sbuf(st2, "g2_%d" % i, [128, D], F32) for i in range(2)]
            lg = kb.sbuf(st2, "lg2", [128, D], F32)
            lb = kb.sbuf(st2, "lb2", [128, D], F32)
            dg = kb.sbuf(st2, "dg", [128, 128], F32)
            psbc = kb.psum(st2, "psbc", [128, 1024], F32)
            for seg in range(2):
                bcast_from_modT(kb, dg, psbc, C, 40, seg, g2[seg][:, :])
            for nm, dst in (("ln2_g", lg), ("ln2_b", lb)):
                kb.dma(kb.sp, dst, View(G[nm], G[nm].ap[l].partition_broadcast(128)))
            residual_ln(kb, C, st2, None, g2, None, lg, lb, acc, out_final=(G["out"] if last else None), ntile=ntl)
            kb.barrier()


def phase_conf_sgu(kb, C, G, l):
    with ExitStack() as st:
        lockstep([phase_conformer_g(kb, C, G, l, st), phase_sgu_g(kb, C, G, l, st)])
        kb.barrier()


def phase_inproj_g(kb, C, G, l, xT, st, prog):
    wt = [kb.sbuf(st, "wi%d" % i, [128, 8, 128], BF16) for i in range(3)]
    stg = [kb.sbuf(st, "stg%d" % i, [128, NT], F32) for i in range(2)]
    bi = kb.sbuf(st, "bi", [128, NCH], F32)
    pp = [kb.psum(st, "pp%d" % i, [128, 512], F32) for i in range(2)]
    L = lnT_alloc(kb, st, C, G, l, 0, None, npst=2)
    kb.dma(kb.sp, bi, G["b_inT"][l])
    n = 0
    order = [68] + list(range(68))
    for ci, ch in enumerate(order):
        w = wt[ci % 3]
        kb.dma(kb.pool, w, G["w_inR"][l, ch])
        rows = 128 if ch < 68 else 16
        sg = stg[ci % 2]
        for (t0, tl) in TBLK:
            if ci == 0:
                lnT_tiles(kb, C, L, range(t0 // 128, (t0 + tl) // 128), xT)
            p = pp[n % 2]
            n += 1
            for k in range(8):
                kb.mm(p[0:rows, 0:tl], w[:, k, 0:rows], xT[:, k, t0:t0 + tl], start=(k == 0), stop=(k == 7))
            kb.actf(sg[0:rows, t0:t0 + tl], p[0:rows, 0:tl], AF.Identity, bias=bi[0:rows, ch:ch + 1])
            yield
        kb.dma(kb.sp, C.pT[ch][0:rows, :], sg[0:rows, :])
        prog["ch"] = ci + 1
        yield


def phase_inproj_gdnA(kb, C, G, l, GD):
    with ExitStack() as st:
        xT = kb.sbuf(st, "xT1", [128, 8, NT], BF16)
        prog = {"ch": 0}
        gi = phase_inproj_g(kb, C, G, l, xT, st, prog)
        while prog["ch"] < 13:
            next(gi)
        ga = gdn_stepA_g(kb, C, G, l, GD, st)
        done_i = done_a = False
        while not (done_i and done_a):
            if not done_i:
                try:
                    next(gi)
                except StopIteration:
                    done_i = True
            for _ in range(3):
                if not done_a:
                    try:
                        next(ga)
                    except StopIteration:
                        done_a = True
        kb.barrier()


def phase_mod_g(kb, C, G, l, st):
    modT, onep = C.modT_all[l], C.onep_all[l]
    cl = kb.sbuf(st, "cl", [128, 8], F32)
    cc = kb.sbuf(st, "cc", [128, 8], F32)
    sc = kb.sbuf(st, "sc", [128, 8, 2], F32)
    bm = kb.sbuf(st, "bm", [128, 48], F32)
    wm = [kb.sbuf(st, "wm%d" % i, [128, 8, 768], F32) for i in range(2)]
    ps = kb.psum(st, "psmod", [128, 48, 2], F32)
    kb.dma(kb.sp, cl, G["cT"])
    kb.dma(kb.sp, cc, G["cctxT"])
    kb.dma(kb.sp, bm, G["b_modT"][l])
    kb.actf(sc[:, :, 0], cl[:, :], AF.Silu)
    kb.actf(sc[:, :, 1], cc[:, :], AF.Silu)
    yield
    wsrc = G["w_mod"][l].re("(k p) n -> p k n", p=128)
    for ng in range(8):
        w = wm[ng % 2]
        kb.dma(kb.sp, w, wsrc[:, :, ng * 768:(ng + 1) * 768])
        yield
        for j in range(6):
            ch = ng * 6 + j
            for k in range(8):
                kb.mm(ps[:, ch, :], w[:, k, j * 128:(j + 1) * 128], sc[:, k, :], start=(k == 0), stop=(k == 7))
            yield
    kb.tt(modT[:, :, :], ps[:, :, :], View(bm, bm.ap.unsqueeze(2).broadcast_to([128, 48, 2])), ALU.add)
    yield
    kb.ts(onep[:, 0:8, :], modT[:, 8:16, :], 1.0, None, ALU.add)
    kb.ts(onep[:, 8:16, :], modT[:, 32:40, :], 1.0, None, ALU.add)
    yield


def phase_h0_g(kb, C, G, st):
    xt = [kb.sbuf(st, "xt%d" % i, [128, D], F32) for i in range(2)]
    pt = [kb.sbuf(st, "pt%d" % i, [128, D], F32) for i in range(2)]
    for t in range(NTILE):
        a = xt[t % 2]
        if t < 16:
            b_ = pt[t % 2]
            kb.dma(kb.sp, a, G["x"][t * 128:(t + 1) * 128, :])
            kb.dma(kb.sp, b_, G["pos"][t * 128:(t + 1) * 128, :])
            kb.tt(a[:, :], a[:, :], b_[:, :], ALU.add)
        else:
            kb.dma(kb.sp, a, G["ctx"][(t - 16) * 128:(t - 15) * 128, :])
        kb.dma(kb.sp, C.h[t], a)
        yield


def phase_h0_mod0(kb, C, G):
    with ExitStack() as st:
        lockstep([phase_h0_g(kb, C, G, st), phase_mod_g(kb, C, G, 0, st)])
        kb.barrier()


def phase_conf_sgu_mod(kb, C, G, l, lnext):
    with ExitStack() as st:
        gens = [phase_conformer_g(kb, C, G, l, st, npc=1), phase_sgu_g(kb, C, G, l, st)]
        gm = phase_mod_g(kb, C, G, lnext, st)
        r = 0
        mod_live = True
        while gens or mod_live:
            for g_ in list(gens):
                try:
                    next(g_)
                except StopIteration:
                    gens.remove(g_)
            r += 1
            if mod_live and (r % 15 == 0 or not gens):
                try:
                    next(gm)
                except StopIteration:
                    mod_live = False
        kb.barrier()


def chunk_off(n):
    return n * 64


ORDER_F = [32, 33, 34, 35] + list(range(32))
ORDER_B = [35, 34, 33, 32] + list(range(31, -1, -1))


def bc3(v, shape):
    return View(v.buf, v.ap.broadcast_to(shape))


def gdn_alloc(kb, st0, C, G):
    GD = Ctx()
    GD.qT = kb.sbuf(st0, "qT", [128, 4, NT], BF16)
    GD.kT = kb.sbuf(st0, "kT", [128, 4, NT], BF16)
    GD.vT = kb.sbuf(st0, "vT", [128, 4, NT], BF16)
    GD.gb = kb.sbuf(st0, "gb", [128, 36, 8], F32)
    GD.gm2 = kb.sbuf(st0, "gm2", [128, 3, 64], F32)
    kb.dma(kb.sp, GD.gm2, G["gm2"])
    GD.gml = kb.sbuf(st0, "gml", [128, 5, 128], F32)
    kb.dma(kb.sp, GD.gml, G["gml"])
    return GD


def gdn_stepA_g(kb, C, G, l, GD, st):
    qT, kT, vT, gb = GD.qT, GD.kT, GD.vT, GD.gb
    pad = [kb.sbuf(st, "gpad%d" % i, [128, NT + 8], BF16) for i in range(2)]
    dgc = [kb.sbuf(st, "gdg%d" % i, [128, 5, 128], BF16) for i in range(2)]
    acc = [[kb.sbuf(st, "gacc%d%d" % (a, i), [128, 512], F32) for i in range(2)] for a in range(2)]
    sq = [[kb.sbuf(st, "gsq%d%d" % (a, i), [128, 512], F32) for i in range(2)] for a in range(2)]
    rs = [[kb.sbuf(st, "grs%d%d" % (a, i), [128, 512], F32) for i in range(2)] for a in range(2)]
    cw = kb.sbuf(st, "gcw", [128, 12, 5], F32)
    ab8 = kb.sbuf(st, "ab8", [40, NT], F32)
    ab = kb.sbuf(st, "gab", [8, 4], F32)
    pcv2 = [kb.psum(st, "gpcv%d" % i, [128, 512], F32) for i in range(2)]
    pss2 = [kb.psum(st, "gpss%d" % i, [128, 512], F32) for i in range(2)]
    psg = pss2[0][:, 0:288].re("p (a b) -> p a b", a=36)
    kb.dma(kb.sp, cw, G["gdn_cwT"][l])
    kb.dma(kb.sp, ab[:, 0:2], G["gdn_ab"][l])
    for i in range(2):
        kb.memset(pad[i][:, :], 0.0)
    yield
    poff = [2, 2048 + 6]

    def chunk_g(j):
        par = j % 2
        pd, dg, pcv, pss = pad[par], dgc[par], pcv2[par], pss2[par]
        for si, (t0, tl) in enumerate(SEGS):
            kb.dma(kb.pool, pd[:, poff[si]:poff[si] + tl], C.pT[j][:, t0:t0 + tl])
        yield
        for k in range(5):
            kb.ts(dg[:, k, :], C.identb[:, :], cw[:, j, k:k + 1], None, ALU.mult)
        yield
        dst = (qT, kT, vT)[j // 4]
        h = j % 4
        nb = 0
        for (t0, tl) in TBLK:
            si = 0 if t0 < 2048 else 1
            off = poff[si] - 2 + (t0 - SEGS[si][0])
            for k in range(5):
                kb.mm(pcv[:, 0:tl], dg[:, k, :], pd[:, off + k:off + k + tl], start=(k == 0), stop=(k == 4))
            yield
            if j >= 8:
                kb.actf(dst[:, h, t0:t0 + tl], pcv[:, 0:tl], AF.Silu)
                yield
            else:
                x, q_, r_ = acc[par][nb % 2], sq[par][nb % 2], rs[par][nb % 2]
                nb += 1
                kb.actf(x[:, 0:tl], pcv[:, 0:tl], AF.Silu)
                yield
                kb.tt(q_[:, 0:tl], x[:, 0:tl], x[:, 0:tl], ALU.mult)
                yield
                kb.mm(pss[:, 0:tl], C.ones[:, :], q_[:, 0:tl])
                yield
                kb.ts(r_[:, 0:tl], pss[:, 0:tl], EPS, None, ALU.add)
                yield
                kb.actf(r_[:, 0:tl], r_[:, 0:tl], AF.Sqrt)
                yield
                kb.recip(r_[:, 0:tl], r_[:, 0:tl])
                yield
                if j < 4:
                    kb.stt(dst[:, h, t0:t0 + tl], x[:, 0:tl], 128.0 ** -0.5, r_[:, 0:tl], ALU.mult, ALU.mult)
                else:
                    kb.tt(dst[:, h, t0:t0 + tl], x[:, 0:tl], r_[:, 0:tl], ALU.mult)
                yield

    for j0 in range(0, 12, 2):
        gens = [chunk_g(j0), chunk_g(j0 + 1)]
        while gens:
            for g_ in list(gens):
                try:
                    next(g_)
                except StopIteration:
                    gens.remove(g_)
            yield
    a8, b8 = ab8[0:8, :], ab8[32:40, :]
    kb.dma(kb.sp, a8, C.pT[68][0:8, :])
    kb.dma(kb.sp, b8, C.pT[68][8:16, :])
    yield
    kb.actf(a8, a8, AF.Exp, bias=ab[:, 1:2])
    yield
    kb.ts(a8, a8, 1.0, None, ALU.add)
    yield
    kb.actf(a8, a8, AF.Ln)
    yield
    kb.actf(ab[:, 2:3], ab[:, 0:1], AF.Exp)
    kb.ts(ab[:, 3:4], ab[:, 2:3], -1.0, None, ALU.mult)
    yield
    kb.ts(a8, a8, ab[:, 3:4], None, ALU.mult)
    yield
    kb.actf(b8, b8, AF.Sigmoid)
    yield
    idb_ = C.ident[32:40, 32:40]
    for n in range(36):
        sl = slice(n * 64, n * 64 + 64)
        sf, sb = ORDER_F.index(n), ORDER_B.index(n)
        kb.mm(psg[0:64, sf, 0:4], ab8[0:8, sl], C.ident[0:8, 0:4])
        kb.mm(psg[64:128, sb, 0:4], ab8[0:8, sl], C.ident[0:8, 4:8])
        kb.mm(psg[0:64, sf, 4:8], ab8[32:40, sl], C.ident[32:40, 32:36])
        kb.mm(psg[64:128, sb, 4:8], ab8[32:40, sl], C.ident[32:40, 36:40])
        if n % 4 == 3:
            yield
    kb.copy(gb[:, :, :], psg[:, :, :])
    yield


def gdn_stepBC(kb, C, G, l, GD):
    qT, kT, vT, gb, gm2, gml = GD.qT, GD.kT, GD.vT, GD.gb, GD.gm2, GD.gml
    if True:
        with ExitStack() as st:
            oT = kb.sbuf(st, "oT", [128, 4, NT], BF16)
            S = kb.sbuf(st, "S", [128, 8, 128], F32)
            Sbf = kb.sbuf(st, "Sbf", [128, 8, 128], BF16)
            X = [kb.psum(st, "gX%d" % i, [128, 512], F32) for i in range(3)]
            Yp = [kb.psum(st, "gY%d" % i, [128, 512], F32) for i in range(3)]
            XS = kb.psum(st, "gXS", [128, 1024], F32)
            visited = set()
            stw = ExitStack()
            w = {}
            for name, shape, dt in [("ktok", [128, 4, 128], F32), ("vb", [128, 4, 128], F32), ("kbg", [128, 4, 128], F32),
                                    ("u", [128, 4, 128], F32), ("kd", [128, 4, 128], BF16), ("vn", [128, 4, 128], BF16),
                                    ("obf", [128, 4, 128], BF16), ("sm", [128, 64], F32),
                                    ("G0", [128, 4, 64], F32), ("G1", [128, 4, 64], F32), ("E", [128, 4, 64], F32),
                                    ("ET", [128, 4, 64], F32), ("P", [128, 4, 64], F32), ("Q", [128, 4, 64], F32),
                                    ("Y", [128, 4, 64], F32), ("P2", [128, 4, 64], F32), ("Q2", [128, 4, 64], F32),
                                    ("Y2", [128, 4, 64], F32), ("at", [128, 4, 64], BF16), ("wT", [128, 8, 64], BF16)]:
                w[name] = kb.sbuf(stw, "g" + name, shape, dt)
            kb.memset(S[:, :, :], 0.0, eng=kb.pool)
            kb.memset(Sbf[:, :, :], 0.0, eng=kb.pool)
            HALF = (slice(0, 64), slice(64, 128))
            idf = [C.ident[0:64, 0:64], C.ident[64:128, 64:128]]
            idb = [C.identb[0:64, 0:64], C.identb[64:128, 64:128]]

            def mbc(k):
                return View(gm2, gm2.ap[:, k, :].unsqueeze(1).broadcast_to([128, 4, 64]))
            maskA, maskB, ident4 = mbc(0), mbc(1), mbc(2)

            def fl(v):
                return v.re("p h c -> p (h c)")
            for s in range(36):
                nch = (ORDER_F[s], ORDER_B[s])
                sls = [slice(n * 64, n * 64 + 64) for n in nch]
                g4 = gb[:, s, 0:4]
                beta4 = gb[:, s, 4:8]
                sm = w["sm"]
                egc, ekd, bg, nb_, gcs, egl8 = (sm[:, 0:4], sm[:, 4:8], sm[:, 8:12], sm[:, 12:16], sm[:, 16:20], sm[:, 24:32])
                for h in range(4):
                    for d in range(2):
                        kb.mm(X[0][HALF[d], h * 128:(h + 1) * 128], kT[:, h, sls[d]], C.identb[:, :])
                        kb.mm(X[1][HALF[d], h * 128:(h + 1) * 128], vT[:, h, sls[d]], C.identb[:, :])
                kb.mm(Yp[0][:, 0:4], gml[:, 0, :], g4)
                kb.mm(Yp[0][:, 4:8], gml[:, 2, :], g4)
                kb.mm(Yp[0][:, 8:12], gml[:, 3, :], g4)
                kb.mm(Yp[0][:, 12:16], gml[:, 4, :], g4)
                kb.actf(egc, Yp[0][:, 0:4], AF.Exp)
                kb.actf(egl8, Yp[0][:, 8:16], AF.Exp)
                kb.copy(gcs, Yp[0][:, 0:4], eng=kb.act)
                kb.tt(ekd, Yp[0][:, 4:8], gcs, ALU.subtract)
                kb.actf(ekd, ekd, AF.Exp)
                kb.tt(bg, beta4, egc, ALU.mult)
                kb.ts(nb_, beta4, -1.0, None, ALU.mult)
                k3 = X[0][:, :].re("p (h c) -> p h c", h=4)
                v3 = X[1][:, :].re("p (h c) -> p h c", h=4)
                kb.copy(w["ktok"][:, :, :], k3, eng=kb.act)
                kb.tt(w["vb"][:, :, :], v3, bc3(View(gb, beta4.ap.unsqueeze(2)), [128, 4, 128]), ALU.mult)
                kb.tt(w["kbg"][:, :, :], w["ktok"][:, :, :], bc3(View(sm, bg.ap.unsqueeze(2)), [128, 4, 128]), ALU.mult)
                kb.tt(w["kd"][:, :, :], w["ktok"][:, :, :], bc3(View(sm, ekd.ap.unsqueeze(2)), [128, 4, 128]), ALU.mult, eng=kb.pool)
                gbc = bc3(View(gb, g4.ap.unsqueeze(2)), [128, 4, 64])
                kb.tt(w["G1"][:, :, :], maskA, gbc, ALU.mult)
                kb.tt(w["G0"][:, :, :], maskB, gbc, ALU.mult, eng=kb.pool)
                kb.mm(X[2][:, 0:256], gml[:, 0, :], fl(w["G1"][:, :, :]))
                kb.mm(X[2][:, 256:512], gml[:, 1, :], fl(w["G0"][:, :, :]))
                kb.actf(fl(w["E"][:, :, :]), X[2][:, 0:256], AF.Exp)
                kb.actf(fl(w["ET"][:, :, :]), X[2][:, 256:512], AF.Exp)
                kb.tt(w["E"][:, :, :], w["E"][:, :, :], maskA, ALU.mult)
                kb.tt(w["ET"][:, :, :], w["ET"][:, :, :], maskB, ALU.mult, eng=kb.pool)
                for h in range(4):
                    for d in range(2):
                        kb.mm(Yp[1][HALF[d], h * 64:(h + 1) * 64], kT[:, h, sls[d]], kT[:, h, sls[d]])
                        kb.mm(Yp[0][HALF[d], 256 + h * 64:256 + (h + 1) * 64], kT[:, h, sls[d]], qT[:, h, sls[d]])
                kk3 = Yp[1][:, 0:256].re("p (h c) -> p h c", h=4)
                qk3 = Yp[0][:, 256:512].re("p (h c) -> p h c", h=4)
                kb.tt(w["E"][:, :, :], kk3, w["E"][:, :, :], ALU.mult)
                kb.tt(w["P"][:, :, :], w["E"][:, :, :], bc3(View(sm, nb_.ap.unsqueeze(2)), [128, 4, 64]), ALU.mult)
                for h in range(4):
                    for d in range(2):
                        kb.mm(Yp[1][HALF[d], 256 + h * 64:256 + (h + 1) * 64], w["P"][HALF[d], h, :], idf[d])
                kb.copy(fl(w["Q"][:, :, :]), Yp[1][:, 256:512], eng=kb.act)
                kb.tt(w["Y"][:, :, :], w["Q"][:, :, :], ident4, ALU.add)
                P, Q, Y = w["P"], w["Q"], w["Y"]
                P2, Q2, Y2 = w["P2"], w["Q2"], w["Y2"]
                pendZ = None
                for lev in range(1, 6):
                    for h in range(4):
                        for d in range(2):
                            kb.mm(X[0][HALF[d], h * 64:(h + 1) * 64], Q[HALF[d], h, :], P[HALF[d], h, :])
                    if lev < 5:
                        for h in range(4):
                            for d in range(2):
                                kb.mm(Yp[1][HALF[d], h * 64:(h + 1) * 64], P[HALF[d], h, :], Q[HALF[d], h, :])
                    if pendZ is not None:
                        Pz, Ya, Yb = pendZ
                        for h in range(4):
                            for d in range(2):
                                kb.mm(X[1][HALF[d], h * 64:(h + 1) * 64], Pz[HALF[d], h, :], Ya[HALF[d], h, :])
                    kb.copy(fl(P2[:, :, :]), X[0][:, 0:256], eng=kb.act)
                    if lev < 5:
                        kb.copy(fl(Q2[:, :, :]), Yp[1][:, 0:256])
                    if pendZ is not None:
                        kb.tt(fl(Yb[:, :, :]), fl(Ya[:, :, :]), X[1][:, 0:256], ALU.add)
                    pendZ = (P2, Y, Y2)
                    P, P2 = P2, P
                    Q, Q2 = Q2, Q
                    Y, Y2 = Y2, Y
                Pz, Ya, Yb = pendZ
                for h in range(4):
                    for d in range(2):
                        kb.mm(X[1][HALF[d], h * 64:(h + 1) * 64], Pz[HALF[d], h, :], Ya[HALF[d], h, :])
                kb.tt(fl(Yb[:, :, :]), fl(Ya[:, :, :]), X[1][:, 0:256], ALU.add)
                kb.tt(w["at"][:, :, :], qk3, w["ET"][:, :, :], ALU.mult)
                for h in range(4):
                    for d in range(2):
                        kb.mm(X[2][HALF[d], h * 128:(h + 1) * 128], Y[HALF[d], h, :], w["vb"][HALF[d], h, :])
                        kb.mm((Yp[0], Yp[2])[d][:, h * 64:(h + 1) * 64], w["kbg"][HALF[d], h, :], Y[HALF[d], h, :])
                kb.copy(fl(w["u"][:, :, :]), X[2][:, :], eng=kb.act)
                kb.copy(fl(w["wT"][:, 0:4, :]), Yp[0][:, 0:256])
                kb.copy(fl(w["wT"][:, 4:8, :]), Yp[2][:, 0:256])
                for h in range(4):
                    for d in range(2):
                        kb.mm(X[0][HALF[d], h * 128:(h + 1) * 128], w["wT"][:, d * 4 + h, :], Sbf[:, d * 4 + h, :])
                        kb.mm(X[1][HALF[d], h * 128:(h + 1) * 128], qT[:, h, sls[d]], Sbf[:, d * 4 + h, :])
                kb.tt(fl(w["vn"][:, :, :]), fl(w["u"][:, :, :]), X[0][:, :], ALU.subtract)
                for h in range(4):
                    for d in range(2):
                        kb.mm(X[2][HALF[d], h * 128:(h + 1) * 128], w["at"][HALF[d], h, :], w["vn"][HALF[d], h, :])
                for h in range(4):
                    for d in range(2):
                        kb.mm(XS[:, (d * 4 + h) * 128:(d * 4 + h + 1) * 128], w["kd"][HALF[d], h, :], w["vn"][HALF[d], h, :])
                o1 = w["u"]
                kb.tt(o1[:, :, :], X[1][:, :].re("p (h c) -> p h c", h=4), bc3(View(sm, egc.ap.unsqueeze(2)), [128, 4, 128]), ALU.mult)
                kb.tt(fl(w["obf"][:, :, :]), fl(o1[:, :, :]), X[2][:, :], ALU.add)
                kb.tt(S[:, :, :], S[:, :, :], bc3(View(sm, egl8.ap.unsqueeze(2)), [128, 8, 128]), ALU.mult)
                kb.tt(fl(S[:, :, :]), fl(S[:, :, :]), XS[:, :], ALU.add)
                kb.copy(Sbf[:, :, :], S[:, :, :], eng=kb.act)
                for h in range(4):
                    for d in range(2):
                        kb.mm((Yp[1], Yp[2])[d][:, h * 64:(h + 1) * 64], w["obf"][HALF[d], h, :], idb[d])
                for d in range(2):
                    o3 = (Yp[1], Yp[2])[d][:, 0:256].re("p (h c) -> p h c", h=4)
                    n = nch[d]
                    if n in visited:
                        kb.tt(oT[:, :, sls[d]], oT[:, :, sls[d]], o3, ALU.add)
                    else:
                        kb.copy(oT[:, :, sls[d]], o3, eng=kb.act)
                        visited.add(n)
            kb.barrier()
            stw.close()
            with ExitStack() as st2:
                z = [kb.sbuf(st2, "gz%d" % i, [128, NT], F32) for i in range(2)]
                sq = [kb.sbuf(st2, "gsq2%d" % i, [128, 512], F32) for i in range(2)]
                rs = [kb.sbuf(st2, "grs2%d" % i, [128, 512], F32) for i in range(2)]
                ob = [kb.sbuf(st2, "gob%d" % i, [128, NT], BF16) for i in range(2)]
                nw = kb.sbuf(st2, "gnw", [128, 1], F32)
                kb.dma(kb.sp, nw, G["gdn_norm_w"][l].re("(p o) -> p o", o=1))

                def head_g(h):
                    z_, sq_, rs_, ob_, ps_ = z[h % 2], sq[h % 2], rs[h % 2], ob[h % 2], Yp[h % 2]
                    kb.dma(kb.sp, z_, C.pT[O_Z // 128 + h])
                    yield
                    kb.actf(z_[:, :], z_[:, :], AF.Silu)
                    yield
                    for (t0, tl) in TBLK:
                        x = oT[:, h, t0:t0 + tl]
                        kb.tt(sq_[:, 0:tl], x, x, ALU.mult)
                        yield
                        kb.mm(ps_[:, 0:tl], C.onesm[:, :], sq_[:, 0:tl])
                        yield
                        kb.ts(rs_[:, 0:tl], ps_[:, 0:tl], EPS, None, ALU.add)
                        yield
                        kb.actf(rs_[:, 0:tl], rs_[:, 0:tl], AF.Sqrt)
                        yield
                        kb.recip(rs_[:, 0:tl], rs_[:, 0:tl])
                        yield
                        kb.stt(sq_[:, 0:tl], x, nw[:, 0:1], rs_[:, 0:tl], ALU.mult, ALU.mult)
                        yield
                        kb.tt(ob_[:, t0:t0 + tl], sq_[:, 0:tl], z_[:, t0:t0 + tl], ALU.mult)
                        yield
                    kb.dma(kb.sp, C.brT[0][h], ob_)
                    yield
                for h0 in (0, 2):
                    lockstep([head_g(h0), head_g(h0 + 1)])
                kb.barrier()


def phase_gdn(kb, C, G, l):
    with ExitStack() as st0:
        GD = gdn_alloc(kb, st0, C, G)
        with ExitStack() as st:
            for _ in gdn_stepA_g(kb, C, G, l, GD, st):
                pass
            kb.barrier()
        gdn_stepBC(kb, C, G, l, GD)


def declare_inputs(kb):
    G = {}

    def inp(name, shape, dt=F32):
        G[name] = kb.dram(name, shape, dt, kind="ExternalInput")

    L = 2
    inp("x", [2048, D]); inp("ctx", [256, D]); inp("cT", [128, 8]); inp("cctxT", [128, 8])
    inp("pos", [2048, D]); inp("ident", [128, 128])
    inp("w_mod", [L, D, 6 * D]); inp("b_modT", [L, 128, 48])
    inp("w_inR", [L, NCH, 128, 8, 128]); inp("b_inT", [L, 128, NCH])
    inp("gdn_cwT", [L, 128, 12, 5]); inp("gdn_ab", [L, 8, 2]); inp("gdn_norm_w", [L, 128])
    inp("conf_wT", [L, 128, 4, 31]); inp("conf_vT", [L, 128, 3, 4])
    inp("sgu_vT", [L, 128, 2, 4]); inp("sgu_wsT", [L, 4, 128, 128]); inp("sgu_bs", [L, 4, 128])
    inp("w_branch", [L, 4, 512, D]); inp("w_out", [L, D, D])
    for nm in ("b_out", "ln1_g", "ln1_b", "ln2_g", "ln2_b"):
        inp(nm, [L, D])
    inp("rw", [L, D, 20]); inp("rb", [L, 20])
    inp("w_gate", [L, 16, D, 512]); inp("w_up", [L, 16, D, 512]); inp("w_down", [L, 16, 512, D])
    inp("dft_c", [128, 256]); inp("dft_lat", [2, 2048, 2048]); inp("dft_ctx", [2, 256, 256])
    inp("gmask", [64, 6, 64]); inp("gm2", [128, 3, 64]); inp("gml", [128, 5, 128])
    return G


FUSE_INPROJ_GDNA = False


def build(layers=(0, 1), first=True, last=True, debug=False, phases=None):
    nc = bass.Bass("TRN2", target_bir_lowering=False)
    kb = KB(nc)
    G = declare_inputs(kb)
    C = Ctx()
    kind = "ExternalOutput" if debug else "Internal"
    if first and last:
        C.h = [kb.dram("h%d" % t, [128, D], F32, kind=kind) for t in range(NTILE)]
    else:
        C.h = [kb.dram("h%d" % t, [128, D], F32, kind="ExternalOutput") for t in range(NTILE)]
        if not first:
            C.hin = [kb.dram("hin%d" % t, [128, D], F32, kind="ExternalInput") for t in range(NTILE)]
    C.pT = [kb.dram("pT%d" % c, [128, NT], F32, kind=kind) for c in range(NCH)]
    C.brT = [[kb.dram("brT%d_%d" % (i, c), [128, NT], BF16, kind=kind) for c in range(4)] for i in range(4)]
    C.oT = [kb.dram("oT%d" % c, [128, NT], F32, kind=kind) for c in range(4)]
    G["out"] = kb.dram("out", [2048, D], F32, kind="ExternalOutput")
    with ExitStack() as gst:
        C.gst = gst
        phase_consts(kb, C, G)
        mod_done = set()
        if first and (phases is None):
            phase_h0_mod0(kb, C, G)
            mod_done.add(layers[0])
        elif first:
            phase_h0(kb, C, G)
        else:
            with ExitStack() as st:
                tmp = [kb.sbuf(st, "hcp%d" % i, [128, D], F32) for i in range(2)]
                for t in range(NTILE):
                    kb.dma(kb.sp, tmp[t % 2], C.hin[t])
                    kb.dma(kb.sp, C.h[t], tmp[t % 2])
                kb.barrier()
        for l in layers:
            is_last = last and (l == layers[-1])

            def on(p):
                return phases is None or p in phases
            C.modT, C.onep = C.modT_all[l], C.onep_all[l]
            if on("mod") and l not in mod_done:
                with ExitStack() as stm:
                    for _ in phase_mod_g(kb, C, G, l, stm):
                        pass
                    kb.barrier()
            if FUSE_INPROJ_GDNA and on("inproj") and on("gdn"):
                with ExitStack() as sg0:
                    GD = gdn_alloc(kb, sg0, C, G)
                    phase_inproj_gdnA(kb, C, G, l, GD)
                    gdn_stepBC(kb, C, G, l, GD)
            else:
                if on("inproj"):
                    with ExitStack() as st:
                        xT = kb.sbuf(st, "xT1", [128, 8, NT], BF16)
                        phase_inproj(kb, C, G, l, xT)
                if on("gdn"):
                    phase_gdn(kb, C, G, l)
            li = list(layers).index(l)
            if on("conf") and on("sgu") and phases is None and li + 1 < len(layers):
                phase_conf_sgu_mod(kb, C, G, l, layers[li + 1])
                mod_done.add(layers[li + 1])
            elif on("conf") and on("sgu"):
                phase_conf_sgu(kb, C, G, l)
            else:
                if on("conf"):
                    phase_conformer(kb, C, G, l)
                if on("sgu"):
                    phase_sgu(kb, C, G, l)
            if on("fft"):
                phase_fft(kb, C, G, l)
            if on("merge"):
                phase_merge(kb, C, G, l, is_last)
            if on("moe"):
                phase_moe(kb, C, G, l, is_last)
        kb.finish([G["out"]] + C.h)
    return nc, kb


def _consts():
    c = {}
    rows = 2048 // 64
    row = np.broadcast_to(np.arange(rows, dtype=np.float32)[:, None], (rows, 64)).reshape(-1)
    col = np.broadcast_to(np.arange(64, dtype=np.float32)[None, :], (rows, 64)).reshape(-1)
    quarter = D // 4
    omega = (1.0 / (10000.0 ** (np.arange(quarter, dtype=np.float32) / np.float32(quarter)))).astype(np.float32)

    def enc(pos):
        ang = (pos[:, None] * omega[None, :]).astype(np.float32)
        return np.concatenate([np.sin(ang), np.cos(ang)], -1)
    c["pos"] = np.concatenate([enc(row), enc(col)], -1).astype(np.float32)
    c["ident"] = np.eye(128, dtype=np.float32)

    def dft(n):
        k = np.arange(n, dtype=np.int64)
        ang = 2.0 * np.pi * ((k[:, None] * k[None, :]) % n).astype(np.float64) / n
        s = 1.0 / np.sqrt(n)
        return (np.cos(ang) * s).astype(np.float32), (np.sin(ang) * s).astype(np.float32)
    cc, sc = dft(128)
    c["dft_c"] = np.concatenate([cc, sc], 1)
    ct, stt = dft(2048)
    c["dft_lat"] = np.stack([ct, -stt])
    ct, stt = dft(256)
    c["dft_ctx"] = np.stack([ct, -stt])
    i = np.arange(64)
    k_, j_ = i[:, None], i[None, :]
    m = np.stack([(k_ <= j_), (k_ > j_), (k_ >= j_), (k_ < j_), (k_ == j_), (k_ != j_)], 1).astype(np.float32)
    c["gmask"] = np.ascontiguousarray(m)
    lo, up, le, ge, eye = (k_ > j_), (k_ < j_), (k_ <= j_), (k_ >= j_), (k_ == j_)
    gm2 = np.stack([np.concatenate([lo, up], 0), np.concatenate([le, ge], 0), np.concatenate([eye, eye], 0)], 1)
    c["gm2"] = np.ascontiguousarray(gm2.astype(np.float32))

    def bd(a, b):
        z = np.zeros((128, 128), np.float32)
        z[:64, :64] = a
        z[64:, 64:] = b
        return z
    one = np.ones((64, 64), np.float32)
    self_f = np.zeros((128, 128), np.float32); self_f[:64, :] = 1.0
    self_b = np.zeros((128, 128), np.float32); self_b[64:, :] = 1.0
    c["gml"] = np.ascontiguousarray(np.stack([bd(le, ge), bd(lo, up), bd(one, one), self_f, self_b], 1).astype(np.float32))
    return c


_CONSTS = None
PERM = np.concatenate([np.arange(0, 2048), np.arange(2064, 8720), np.arange(2048, 2064)])


def prep_shared(inp):
    global _CONSTS
    if _CONSTS is None:
        _CONSTS = _consts()
    f = lambda a: np.ascontiguousarray(np.asarray(a, dtype=np.float32))
    S = dict(_CONSTS)
    L = 2
    S["w_mod"] = f(inp["w_mod"])
    S["b_modT"] = f(np.asarray(inp["b_mod"]).reshape(L, 48, 128).transpose(0, 2, 1))
    w = np.asarray(inp["w_in"])[:, :, PERM]
    wp = np.zeros((L, D, NCH * 128), np.float32)
    wp[:, :, :8720] = w
    S["w_inR"] = f(wp.reshape(L, 8, 128, NCH, 128).transpose(0, 3, 2, 1, 4))
    b = np.zeros((L, NCH * 128), np.float32)
    b[:, :8720] = np.asarray(inp["b_in"])[:, PERM]
    S["b_inT"] = f(b.reshape(L, NCH, 128).transpose(0, 2, 1))
    S["gdn_cwT"] = f(np.asarray(inp["gdn_conv_w"]).reshape(L, 5, 12, 128).transpose(0, 3, 2, 1))
    S["gdn_ab"] = f(np.stack([np.asarray(inp["gdn_a_log"]).reshape(L, 8), np.asarray(inp["gdn_dt_bias"]).reshape(L, 8)], -1))
    S["gdn_norm_w"] = f(inp["gdn_norm_w"])
    S["conf_wT"] = f(np.asarray(inp["conf_dw_w"]).reshape(L, 31, 4, 128).transpose(0, 3, 2, 1))
    S["conf_vT"] = f(np.stack([np.asarray(inp[k]).reshape(L, 4, 128) for k in ("conf_dw_b", "conf_ln_g", "conf_ln_b")], 1).transpose(0, 3, 1, 2))
    S["sgu_vT"] = f(np.stack([np.asarray(inp[k]).reshape(L, 4, 128) for k in ("sgu_ln_g", "sgu_ln_b")], 1).transpose(0, 3, 1, 2))
    S["sgu_wsT"] = f(np.asarray(inp["sgu_ws"]).transpose(0, 1, 3, 2))
    S["sgu_bs"] = f(inp["sgu_bs"])
    for k in ("w_branch", "w_out", "b_out", "ln1_g", "ln1_b", "ln2_g", "ln2_b"):
        S[k] = f(inp[k])
    S["rw"] = f(np.concatenate([np.asarray(inp["router_group_w"]), np.asarray(inp["router_expert_w"])], -1))
    S["rb"] = f(np.concatenate([np.asarray(inp["router_group_b"]), np.asarray(inp["router_expert_b"])], -1))
    S["w_gate"] = f(inp["expert_w_gate"]); S["w_up"] = f(inp["expert_w_up"]); S["w_down"] = f(inp["expert_w_down"])
    S["cctxT"] = f(np.asarray(inp["c_ctx"]).reshape(8, 128).T)
    return S


def prep_core(inp, S, b):
    m = dict(S)
    m["x"] = np.ascontiguousarray(np.asarray(inp["x"][b], dtype=np.float32))
    m["ctx"] = np.ascontiguousarray(np.asarray(inp["ctx"][b], dtype=np.float32))
    m["cT"] = np.ascontiguousarray(np.asarray(inp["c"][b], dtype=np.float32).reshape(8, 128).T)
    return m


_NC_CACHE = {}


def kernel(**inputs):
    S = prep_shared(inputs)
    maps = [prep_core(inputs, S, b) for b in range(8)]
    if "full" not in _NC_CACHE:
        _NC_CACHE["full"] = build()[0]
    res = run_bass_kernel_spmd(_NC_CACHE["full"], maps, core_ids=list(range(8)))
    return np.stack([np.asarray(r["out"]) for r in res.results], 0).astype(np.float32)
```

```python
import contextlib
import numpy as np
import concourse.bass as bass
import concourse.mybir as mybir
from concourse.bass_utils import run_bass_kernel_spmd

F32 = mybir.dt.float32
BF16 = mybir.dt.bfloat16
AF = mybir.ActivationFunctionType
ALU = mybir.AluOpType
AX = mybir.AxisListType


class Buf:
    def __init__(self, k, name, ap, dma_sem=False):
        self.k = k
        self.name = name
        self.ap = ap
        self.w = None
        self.r = []
        self.ds = {}
        self.is_psum = False

    def __getitem__(self, key):
        return View(self, self.ap[key])

    def v(self, ap):
        return View(self, ap)


class View:
    def __init__(self, buf, ap):
        self.buf = buf
        self.ap = ap

    def __getitem__(self, key):
        return View(self.buf, self.ap[key])

    def re(self, s, **kw):
        return View(self.buf, self.ap.rearrange(s, **kw))

    def bc(self, shape):
        return View(self.buf, self.ap.broadcast_to(shape))

    def bitcast(self, dt):
        return View(self.buf, self.ap.bitcast(dt))


class Eng:
    def __init__(self, k, name, e):
        self.k = k
        self.name = name
        self.e = e
        self.sem = k.nc.alloc_semaphore(name="s_" + name)
        self.n = 0
        self.seen = {}


class DmaTicket:
    __slots__ = ("buf", "kind", "val")

    def __init__(self, buf, kind, val):
        self.buf = buf
        self.kind = kind
        self.val = val


class EngTicket:
    __slots__ = ("eng", "val")

    def __init__(self, eng, val):
        self.eng = eng
        self.val = val


class KB:
    def __init__(self, nc):
        self.nc = nc
        self.pe = Eng(self, "pe", nc.tensor)
        self.act = Eng(self, "act", nc.scalar)
        self.dve = Eng(self, "dve", nc.vector)
        self.pool = Eng(self, "pool", nc.gpsimd)
        self.sp = Eng(self, "sp", nc.sync)
        self.engs = [self.pe, self.act, self.dve, self.pool, self.sp]
        self.stack = contextlib.ExitStack()
        self.free_dsems = {"hw": [], "sw": []}
        self.live_dsem = {}
        self.n_dsem = 0
        self.uid = 0
        self.ninst = 0

    def _name(self, name):
        self.uid += 1
        return "%s_%d" % (name, self.uid)

    def sbuf(self, stack, name, shape, dt):
        t = stack.enter_context(self.nc.sbuf_tensor(self._name(name), list(shape), dt))
        b = Buf(self, name, t[tuple(slice(None) for _ in shape)])
        stack.callback(self._release, b)
        return b

    def psum(self, stack, name, shape, dt=F32):
        assert dt == F32
        free = 1
        for d_ in shape[1:]:
            free *= d_
        nb = (free + 511) // 512
        t = stack.enter_context(self.nc.psum_tensor(self._name(name), [128, nb * 512], dt))
        ap = t[0:shape[0], 0:free]
        if len(shape) == 3:
            ap = ap.rearrange("p (a b) -> p a b", a=shape[1])
        elif len(shape) == 4:
            ap = ap.rearrange("p (a b c) -> p a b c", a=shape[1], b=shape[2])
        b = Buf(self, name, ap)
        b.is_psum = True
        stack.callback(self._release, b)
        return b

    def dram(self, name, shape, dt, kind="Internal"):
        t = self.nc.dram_tensor(name, list(shape), dt, kind=kind)
        return Buf(self, name, t.ap())

    def _release(self, b):
        for kind, (sem, n) in b.ds.items():
            self.free_dsems[kind].append((sem, n))
        b.ds = {}
        self.live_dsem.pop(id(b), None)

    def _get_dsem(self, b, kind):
        if kind not in b.ds:
            if self.free_dsems[kind]:
                sem, n = self.free_dsems[kind].pop()
            else:
                self.n_dsem += 1
                sem, n = self.nc.alloc_semaphore(name="d%s_%d" % (kind, self.n_dsem)), 0
            b.ds[kind] = [sem, n]
            self.live_dsem[id(b)] = b
        return b.ds[kind]

    def barrier(self):
        for e in self.engs:
            for f in self.engs:
                if f is not e and f.n > 0:
                    self._wait(e, EngTicket(f, f.n))
            for b in self.live_dsem.values():
                for kind, (sem, n) in b.ds.items():
                    if n > 0:
                        self._wait(e, DmaTicket(b, kind, 16 * n))

    def _wait(self, eng, t):
        if isinstance(t, EngTicket):
            sem, val, key = t.eng.sem, t.val, ("e", t.eng.name)
        else:
            if t.kind not in t.buf.ds:
                return
            sem, n = t.buf.ds[t.kind]
            val = 16 * n
            key = ("d", id(sem))
        if eng.seen.get(key, 0) >= val:
            return
        eng.seen[key] = val
        eng.e.wait_ge(sem, val)
        self.ninst += 1

    def _deps(self, eng, reads, writes, is_pe=False):
        need = []
        for b in reads:
            if b.w is not None:
                need.append(b.w)
            if b.is_psum:
                for t in b.r:
                    if isinstance(t, EngTicket) and t.eng is not eng:
                        need.append(t)
        for b in writes:
            if b.w is not None:
                need.append(b.w)
            for t in b.r:
                need.append(t)
        for t in need:
            if is_pe and isinstance(t, EngTicket) and t.eng is eng:
                continue
            self._wait(eng, t)

    def _mark(self, ticket, reads, writes):
        for b in writes:
            b.w = ticket
            b.r = []
        for b in reads:
            if b in writes:
                continue
            b.r.append(ticket)
            if len(b.r) > 24:
                d = {}
                for t in b.r:
                    key = ("e", t.eng.name) if isinstance(t, EngTicket) else ("d", id(t.buf), t.kind)
                    if key not in d or d[key].val < t.val:
                        d[key] = t
                b.r = list(d.values())

    def op(self, eng, fn, reads, writes):
        rb = [x.buf if isinstance(x, View) else x for x in reads]
        wb = [x.buf if isinstance(x, View) else x for x in writes]
        self._deps(eng, rb, wb, is_pe=(eng is self.pe))
        ins = fn()
        eng.n += 1
        ins.then_inc(eng.sem, 1)
        self.ninst += 1
        self._mark(EngTicket(eng, eng.n), rb, wb)
        return ins

    def dma(self, q, out, in_, **kw):
        ov = out if isinstance(out, View) else out[...]
        iv = in_ if isinstance(in_, View) else in_[...]
        ob, ib = ov.buf, iv.buf
        self._deps(q, [ib], [ob])
        owner = ob
        if str(ob.ap.space).upper().find("DRAM") >= 0 and str(ib.ap.space).upper().find("DRAM") < 0:
            owner = ib
        kind = "sw" if q is self.pool else "hw"
        ds = self._get_dsem(owner, kind)
        ins = q.e.dma_start(out=ov.ap, in_=iv.ap, **kw)
        ds[1] += 1
        ins.then_inc(ds[0], 16)
        self.ninst += 1
        self._mark(DmaTicket(owner, kind, 16 * ds[1]), [ib], [ob])
        return ins

    def mm(self, out, lhsT, rhs, start=True, stop=True, **kw):
        return self.op(self.pe, lambda: self.nc.tensor.matmul(out.ap, lhsT.ap, rhs.ap, start=start, stop=stop, **kw),
                       [lhsT, rhs], [out])

    def actf(self, out, in_, func, bias=None, scale=None, eng=None, **kw):
        reads = [in_]
        args = {}
        if bias is not None:
            if isinstance(bias, View):
                reads.append(bias)
                args["bias"] = bias.ap
            else:
                args["bias"] = bias
        if scale is not None:
            if isinstance(scale, View):
                reads.append(scale)
                args["scale"] = scale.ap
            else:
                args["scale"] = scale
        writes = [out]
        if "accum_out" in kw and kw["accum_out"] is not None:
            writes.append(kw["accum_out"])
            kw["accum_out"] = kw["accum_out"].ap
        return self.op(self.act, lambda: self.nc.scalar.activation(out=out.ap, in_=in_.ap, func=func, **args, **kw),
                       reads, writes)

    def _veng(self, eng):
        eng = eng or self.dve
        return eng, eng.e

    def tt(self, out, in0, in1, op, eng=None):
        eng, e = self._veng(eng)
        return self.op(eng, lambda: e.tensor_tensor(out=out.ap, in0=in0.ap, in1=in1.ap, op=op), [in0, in1], [out])

    def ts(self, out, in0, s1, s2, op0, op1=None, eng=None, accum_out=None):
        eng, e = self._veng(eng)
        reads = [in0]
        a1 = s1
        a2 = s2
        if isinstance(s1, View):
            reads.append(s1)
            a1 = s1.ap
        if isinstance(s2, View):
            reads.append(s2)
            a2 = s2.ap
        kw = {}
        writes = [out]
        if op1 is not None:
            kw["op1"] = op1
        if accum_out is not None:
            kw["accum_out"] = accum_out.ap
            writes.append(accum_out)
        return self.op(eng, lambda: e.tensor_scalar(out=out.ap, in0=in0.ap, scalar1=a1, scalar2=a2, op0=op0, **kw),
                       reads, writes)

    def stt(self, out, in0, scalar, in1, op0, op1, eng=None):
        eng, e = self._veng(None)
        reads = [in0, in1]
        a = scalar
        if isinstance(scalar, View):
            reads.append(scalar)
            a = scalar.ap
        return self.op(eng, lambda: e.scalar_tensor_tensor(out=out.ap, in0=in0.ap, scalar=a, in1=in1.ap, op0=op0, op1=op1),
                       reads, [out])

    def copy(self, out, in_, eng=None):
        eng, e = self._veng(eng)
        if eng is self.act:
            return self.op(eng, lambda: self.nc.scalar.copy(out=out.ap, in_=in_.ap), [in_], [out])
        return self.op(eng, lambda: e.tensor_copy(out=out.ap, in_=in_.ap), [in_], [out])

    def memset(self, out, val, eng=None):
        eng, e = self._veng(eng)
        return self.op(eng, lambda: e.memset(out.ap, val), [], [out])

    def reduce(self, out, in_, op, axis=AX.X, eng=None):
        eng, e = self._veng(eng)
        return self.op(eng, lambda: e.tensor_reduce(out=out.ap, in_=in_.ap, axis=axis, op=op), [in_], [out])

    def recip(self, out, in_, eng=None):
        eng, e = self._veng(eng)
        return self.op(eng, lambda: e.reciprocal(out=out.ap, in_=in_.ap), [in_], [out])

    def finish(self, out_bufs):
        for b in out_bufs:
            if b.w is not None:
                self._wait(self.sp, b.w)
        for e in self.engs:
            if e is not self.sp and e.n > 0:
                self._wait(self.sp, EngTicket(e, e.n))

ExitStack = contextlib.ExitStack
D = 1024
NT = 2304
NTILE = 18
TBLK = [(0, 512), (512, 512), (1024, 512), (1536, 512), (2048, 256)]
SEGS = [(0, 2048), (2048, 256)]
ALPHA = 4 ** 0.25
EPS = 1e-6
O_Q, O_K, O_V, O_Z, O_CA, O_CG, O_F, O_SU, O_SV, O_G, O_AB = 0, 512, 1024, 1536, 2048, 2560, 3072, 3584, 4096, 4608, 8704
NCH = 69
GELU_C = 1.5957691216057308


def seg_of_tile(t):
    return 0 if t < 16 else 1


class Ctx:
    pass


def lockstep(gens):
    gens = list(gens)
    while gens:
        for g_ in list(gens):
            try:
                next(g_)
            except StopIteration:
                gens.remove(g_)


def ln_stats_g(kb, src, xc, junk, small):
    s, nm, ss, rstd = small[:, 0:1], small[:, 1:2], small[:, 2:3], small[:, 3:4]
    kb.actf(junk, src, AF.Identity, accum_out=s)
    yield
    kb.ts(nm, s, -1.0 / D, None, ALU.mult)
    yield
    kb.actf(xc, src, AF.Identity, bias=nm)
    yield
    kb.actf(junk, xc, AF.Square, accum_out=ss)
    yield
    kb.ts(rstd, ss, 1.0 / D, EPS, ALU.mult, ALU.add)
    yield
    kb.actf(rstd, rstd, AF.Sqrt)
    yield
    kb.recip(rstd, rstd)
    yield


def ln_stats(kb, st, src, xc, junk, small):
    s, nm, ss, rstd = small[:, 0:1], small[:, 1:2], small[:, 2:3], small[:, 3:4]
    kb.actf(junk, src, AF.Identity, accum_out=s)
    kb.ts(nm, s, -1.0 / D, None, ALU.mult)
    kb.actf(xc, src, AF.Identity, bias=nm)
    kb.actf(junk, xc, AF.Square, accum_out=ss)
    kb.ts(rstd, ss, 1.0 / D, EPS, ALU.mult, ALU.add)
    kb.actf(rstd, rstd, AF.Sqrt)
    kb.recip(rstd, rstd)
    return rstd


def phase_consts(kb, C, G):
    nc = kb.nc
    st = C.gst
    C.ident = kb.sbuf(st, "ident", [128, 128], F32)
    kb.dma(kb.sp, C.ident, G["ident"])
    C.identb = kb.sbuf(st, "identb", [128, 128], BF16)
    kb.copy(C.identb[:, :], C.ident[:, :])
    C.ones = kb.sbuf(st, "ones", [128, 128], F32)
    kb.memset(C.ones[:, :], 1.0)
    C.onesm = kb.sbuf(st, "onesm", [128, 128], F32)
    kb.memset(C.onesm[:, :], 1.0 / 128)
    C.modT_all = [kb.sbuf(st, "modT%d" % i, [128, 48, 2], F32) for i in range(2)]
    C.onep_all = [kb.sbuf(st, "onep%d" % i, [128, 16, 2], F32) for i in range(2)]
    C.modT, C.onep = C.modT_all[0], C.onep_all[0]
    C.small = kb.sbuf(st, "small", [128, 4], F32)


def phase_h0(kb, C, G):
    with ExitStack() as st:
        xt = [kb.sbuf(st, "xt%d" % i, [128, D], F32) for i in range(2)]
        pt = [kb.sbuf(st, "pt%d" % i, [128, D], F32) for i in range(2)]
        for t in range(NTILE):
            a = xt[t % 2]
            if t < 16:
                b = pt[t % 2]
                kb.dma(kb.sp, a, G["x"][t * 128:(t + 1) * 128, :])
                kb.dma(kb.sp, b, G["pos"][t * 128:(t + 1) * 128, :])
                kb.tt(a[:, :], a[:, :], b[:, :], ALU.add)
            else:
                kb.dma(kb.sp, a, G["ctx"][(t - 16) * 128:(t - 15) * 128, :])
            kb.dma(kb.sp, C.h[t], a)
        kb.barrier()


def phase_mod(kb, C, G, l):
    with ExitStack() as st:
        cl = kb.sbuf(st, "cl", [128, 8], F32)
        cc = kb.sbuf(st, "cc", [128, 8], F32)
        sc = kb.sbuf(st, "sc", [128, 8, 2], F32)
        bm = kb.sbuf(st, "bm", [128, 48], F32)
        wm = [kb.sbuf(st, "wm%d" % i, [128, 8, 768], F32) for i in range(2)]
        ps = kb.psum(st, "psmod", [128, 48, 2], F32)
        kb.dma(kb.sp, cl, G["cT"])
        kb.dma(kb.sp, cc, G["cctxT"])
        kb.dma(kb.sp, bm, G["b_modT"][l])
        kb.actf(sc[:, :, 0], cl[:, :], AF.Silu)
        kb.actf(sc[:, :, 1], cc[:, :], AF.Silu)
        wsrc = G["w_mod"][l].re("(k p) n -> p k n", p=128)
        for ng in range(8):
            w = wm[ng % 2]
            kb.dma(kb.sp, w, wsrc[:, :, ng * 768:(ng + 1) * 768])
            for j in range(6):
                ch = ng * 6 + j
                for k in range(8):
                    kb.mm(ps[:, ch, :], w[:, k, j * 128:(j + 1) * 128], sc[:, k, :], start=(k == 0), stop=(k == 7))
        kb.tt(C.modT[:, :, :], ps[:, :, :], View(bm, bm.ap.unsqueeze(2).broadcast_to([128, 48, 2])), ALU.add)
        kb.ts(C.onep[:, 0:8, :], C.modT[:, 8:16, :], 1.0, None, ALU.add)
        kb.ts(C.onep[:, 8:16, :], C.modT[:, 32:40, :], 1.0, None, ALU.add)
        kb.barrier()


def bcast_from_modT(kb, dg, ps, C, idx, seg, out):
    for c in range(8):
        kb.ts(dg[:, :], C.ident[:, :], C.modT[:, idx + c, seg:seg + 1], None, ALU.mult)
        kb.mm(ps[:, c * 128:(c + 1) * 128], C.ones[:, :], dg[:, :])
    kb.copy(out, ps[:, :])


def lnT_alloc(kb, st, C, G, l, which, router=None, npst=2):
    L = Ctx()
    L.sh_idx = 0 if which == 0 else 24
    L.op_idx = 0 if which == 0 else 8
    L.ht = [kb.sbuf(st, "ht%d" % i, [128, D], F32) for i in range(2)]
    L.xc = [kb.sbuf(st, "xc%d" % i, [128, D], F32) for i in range(2)]
    L.junk2 = [kb.sbuf(st, "junk%d" % i, [128, D], F32) for i in range(2)]
    L.sm = [kb.sbuf(st, "sm%d" % i, [128, 4], F32) for i in range(2)]
    L.pst = [kb.psum(st, "pst%d" % i, [128, 8, 128], F32) for i in range(npst)]
    if router is not None:
        L.x32 = [kb.sbuf(st, "x32%d" % i, [128, 8, 128], F32) for i in range(2)]
        L.rw = kb.sbuf(st, "rw", [128, 8, 20], F32)
        L.rb = kb.sbuf(st, "rb", [1, 20], F32)
        kb.dma(kb.sp, L.rw, G["rw"][l].re("(k p) n -> p k n", p=128))
        kb.dma(kb.sp, L.rb, G["rb"][l:l + 1, :])
        L.plg = [kb.psum(st, "plg%d" % i, [128, 32], F32) for i in range(2)]
        L.R2 = [{k: kb.sbuf(st, "r%d_" % i + k, [128, n], F32) for k, n in
                 [("lg", 20), ("oh", 4), ("ex", 4), ("t16", 16), ("es", 4), ("k1", 4), ("e2", 4), ("k2", 4),
                  ("t4", 4), ("cs", 4), ("s", 12)]} for i in range(2)]
    return L


def lnT_tile_g(kb, C, L, t, xT, router=None):
    seg = seg_of_tile(t)
    h, x, s_, p = L.ht[t % 2], L.xc[t % 2], L.sm[t % 2], L.pst[t % len(L.pst)]
    junk = L.junk2[t % 2]
    kb.dma(kb.sp, h, C.h[t])
    yield
    yield from ln_stats_g(kb, h[:, :], x[:, :], junk[:, :], s_)
    kb.actf(x[:, :], x[:, :], AF.Identity, scale=s_[:, 3:4])
    yield
    for c in range(8):
        kb.mm(p[:, c, :], x[:, c * 128:(c + 1) * 128], C.ident[:, :])
    yield
    for c in range(8):
        dst = L.x32[t % 2][:, c, :] if router is not None else xT[:, c, t * 128:(t + 1) * 128]
        kb.actf(dst, p[:, c, :], AF.Identity, bias=C.modT[:, L.sh_idx + c, seg:seg + 1],
                scale=C.onep[:, L.op_idx + c, seg:seg + 1])
        if c % 4 == 3:
            yield
    if router is not None:
        xx = L.x32[t % 2]
        kb.copy(xT[:, :, t * 128:(t + 1) * 128], xx[:, :, :])
        lg = L.plg[t % 2]
        for c in range(8):
            kb.mm(lg[:, 0:20], xx[:, c, :], L.rw[:, c, :], start=(c == 0), stop=False)
        kb.mm(lg[:, 0:20], C.ones[0:1, :], L.rb[0:1, :], start=False, stop=True)
        yield
        yield from route_g(kb, L.R2[t % 2], lg, router[:, t, :])


def lnT_tile(kb, C, L, t, xT, router=None):
    for _ in lnT_tile_g(kb, C, L, t, xT, router):
        pass


def lnT_tiles(kb, C, L, tiles, xT, router=None):
    tiles = list(tiles)
    for i in range(0, len(tiles), 2):
        lockstep([lnT_tile_g(kb, C, L, t, xT, router) for t in tiles[i:i + 2]])


def phase_lnT(kb, C, G, l, which, xT, router=None, ntile=NTILE):
    with ExitStack() as st:
        L = lnT_alloc(kb, st, C, G, l, which, router)
        lnT_tiles(kb, C, L, range(ntile), xT, router)
        kb.barrier()


def route_g(kb, R, lg, comb):
    s = R["s"]
    gmax, ngmax, gs, gp, m1, m2, dd, ex2, w1, w2 = (s[:, i:i + 1] for i in range(10))
    L = R["lg"]
    kb.copy(L[:, :], lg[:, 0:20])
    yield
    kb.reduce(gmax, L[:, 0:4], ALU.max)
    yield
    kb.ts(R["oh"][:, :], L[:, 0:4], gmax, None, ALU.is_equal)
    yield
    kb.ts(ngmax, gmax, -1.0, None, ALU.mult)
    yield
    kb.actf(R["ex"][:, :], L[:, 0:4], AF.Exp, bias=ngmax, accum_out=gs)
    yield
    kb.recip(gp, gs)
    yield
    oh = R["oh"]
    kb.tt(R["t16"][:, :].re("p (g e) -> p g e", g=4), L[:, 4:20].re("p (g e) -> p g e", g=4),
          View(oh, oh.ap.unsqueeze(2).broadcast_to([128, 4, 4])), ALU.mult)
    yield
    kb.reduce(R["es"][:, :], R["t16"][:, :].re("p (g e) -> p e g", g=4), ALU.add)
    yield
    kb.reduce(m1, R["es"][:, :], ALU.max)
    yield
    kb.ts(R["k1"][:, :], R["es"][:, :], m1, None, ALU.is_equal)
    yield
    kb.stt(R["e2"][:, :], R["k1"][:, :], -1e30, R["es"][:, :], ALU.mult, ALU.add)
    yield
    kb.reduce(m2, R["e2"][:, :], ALU.max)
    yield
    kb.ts(R["k2"][:, :], R["e2"][:, :], m2, None, ALU.is_equal)
    yield
    kb.tt(dd, m2, m1, ALU.subtract)
    yield
    kb.actf(ex2, dd, AF.Exp)
    yield
    kb.ts(w1, ex2, 1.0, None, ALU.add)
    yield
    kb.recip(w1, w1)
    yield
    kb.tt(w2, ex2, w1, ALU.mult)
    yield
    kb.tt(w1, w1, gp, ALU.mult)
    yield
    kb.tt(w2, w2, gp, ALU.mult)
    yield
    kb.ts(R["t4"][:, :], R["k1"][:, :], w1, None, ALU.mult)
    yield
    kb.stt(R["cs"][:, :], R["k2"][:, :], w2, R["t4"][:, :], ALU.mult, ALU.add)
    yield
    cs = R["cs"]
    kb.tt(comb.re("p (g e) -> p g e", g=4), View(oh, oh.ap.unsqueeze(2).broadcast_to([128, 4, 4])),
          View(cs, cs.ap.unsqueeze(1).broadcast_to([128, 4, 4])), ALU.mult)
    yield


def phase_inproj(kb, C, G, l, xT):
    with ExitStack() as st:
        wt = [kb.sbuf(st, "wi%d" % i, [128, 8, 128], BF16) for i in range(3)]
        stg = [kb.sbuf(st, "stg%d" % i, [128, NT], F32) for i in range(2)]
        bi = kb.sbuf(st, "bi", [128, NCH], F32)
        pp = [kb.psum(st, "pp%d" % i, [128, 512], F32) for i in range(4)]
        L = lnT_alloc(kb, st, C, G, l, 0, None)
        kb.dma(kb.sp, bi, G["b_inT"][l])
        n = 0
        for ch in range(NCH):
            w = wt[ch % 3]
            kb.dma(kb.pool, w, G["w_inR"][l, ch])
            rows = 128 if ch < 68 else 16
            sg = stg[ch % 2]
            for (t0, tl) in TBLK:
                if ch == 0:
                    lnT_tiles(kb, C, L, range(t0 // 128, (t0 + tl) // 128), xT)
                p = pp[n % 4]
                n += 1
                for k in range(8):
                    kb.mm(p[0:rows, 0:tl], w[:, k, 0:rows], xT[:, k, t0:t0 + tl], start=(k == 0), stop=(k == 7))
                kb.actf(sg[0:rows, t0:t0 + tl], p[0:rows, 0:tl], AF.Identity, bias=bi[0:rows, ch:ch + 1])
            kb.dma(kb.sp, C.pT[ch][0:rows, :], sg[0:rows, :])
        kb.barrier()


def group_ln_fm(kb, C, st, x, out, n, gcol, bcol, tmp, psA, psB):
    sq, r = tmp
    kb.actf(sq[:, 0:n], x, AF.Square)
    kb.mm(psA[:, 0:n], C.onesm[:, :], x)
    kb.mm(psB[:, 0:n], C.onesm[:, :], sq[:, 0:n])
    kb.actf(sq[:, 0:n], psA[:, 0:n], AF.Square)
    kb.tt(r[:, 0:n], psB[:, 0:n], sq[:, 0:n], ALU.subtract)
    kb.ts(r[:, 0:n], r[:, 0:n], EPS, None, ALU.add)
    kb.actf(r[:, 0:n], r[:, 0:n], AF.Sqrt)
    kb.recip(r[:, 0:n], r[:, 0:n])
    kb.tt(sq[:, 0:n], x, psA[:, 0:n], ALU.subtract)
    kb.tt(sq[:, 0:n], sq[:, 0:n], r[:, 0:n], ALU.mult)
    kb.ts(out, sq[:, 0:n], gcol, bcol, ALU.mult, ALU.add)


def group_ln_fm_g(kb, C, st, x, out, n, gcol, bcol, tmp, psA, psB):
    sq, r = tmp
    kb.actf(sq[:, 0:n], x, AF.Square)
    yield
    kb.mm(psA[:, 0:n], C.onesm[:, :], x)
    yield
    kb.mm(psB[:, 0:n], C.onesm[:, :], sq[:, 0:n])
    yield
    kb.actf(sq[:, 0:n], psA[:, 0:n], AF.Square)
    yield
    kb.tt(r[:, 0:n], psB[:, 0:n], sq[:, 0:n], ALU.subtract)
    yield
    kb.ts(r[:, 0:n], r[:, 0:n], EPS, None, ALU.add)
    yield
    kb.actf(r[:, 0:n], r[:, 0:n], AF.Sqrt)
    yield
    kb.recip(r[:, 0:n], r[:, 0:n])
    yield
    kb.tt(sq[:, 0:n], x, psA[:, 0:n], ALU.subtract)
    yield
    kb.tt(sq[:, 0:n], sq[:, 0:n], r[:, 0:n], ALU.mult)
    yield
    kb.ts(out, sq[:, 0:n], gcol, bcol, ALU.mult, ALU.add)
    yield


def phase_conformer(kb, C, G, l):
    with ExitStack() as st:
        a = [kb.sbuf(st, "ca%d" % i, [128, NT], F32) for i in range(2)]
        g = [kb.sbuf(st, "cg%d" % i, [128, NT], F32) for i in range(2)]
        pad = [kb.sbuf(st, "cpad%d" % i, [128, NT + 60], BF16) for i in range(2)]
        dg = [kb.sbuf(st, "cdg%d" % i, [128, 31, 128], BF16) for i in range(2)]
        ob = kb.sbuf(st, "cob", [128, NT], BF16)
        tmp = [kb.sbuf(st, "ctmp%d" % i, [128, 512], F32) for i in range(2)]
        lnx = [kb.sbuf(st, "clnx%d" % i, [128, 512], F32) for i in range(2)]
        cw = kb.sbuf(st, "cw", [128, 4, 31], F32)
        cv = kb.sbuf(st, "cv", [128, 3, 4], F32)
        psA = kb.psum(st, "cpsA", [128, 512], F32)
        psB = kb.psum(st, "cpsB", [128, 512], F32)
        pc = [kb.psum(st, "cpc%d" % i, [128, 512], F32) for i in range(2)]
        kb.dma(kb.sp, cw, G["conf_wT"][l])
        kb.dma(kb.sp, cv, G["conf_vT"][l])
        for i in range(2):
            kb.memset(pad[i][:, :], 0.0)
        poff = [15, 2048 + 45]
        nb = 0
        for ch in range(4):
            a_, g_, pad_, dg_ = a[ch % 2], g[ch % 2], pad[ch % 2], dg[ch % 2]
            kb.dma(kb.sp, a_, C.pT[O_CA // 128 + ch])
            kb.dma(kb.sp, g_, C.pT[O_CG // 128 + ch])
            kb.actf(g_[:, :], g_[:, :], AF.Sigmoid)
            for si, (t0, tl) in enumerate(SEGS):
                kb.tt(pad_[:, poff[si]:poff[si] + tl], a_[:, t0:t0 + tl], g_[:, t0:t0 + tl], ALU.mult)
            for k in range(31):
                kb.ts(dg_[:, k, :], C.identb[:, :], cw[:, ch, k:k + 1], None, ALU.mult)
            for (t0, tl) in TBLK:
                si = 0 if t0 < 2048 else 1
                off = poff[si] - 15 + (t0 - SEGS[si][0])
                p = pc[nb % 2]
                x = lnx[nb % 2]
                nb += 1
                for k in range(31):
                    kb.mm(p[:, 0:tl], dg_[:, k, :], pad_[:, off + k:off + k + tl], start=(k == 0), stop=(k == 30))
                kb.actf(x[:, 0:tl], p[:, 0:tl], AF.Identity, bias=cv[:, 0, ch:ch + 1])
                group_ln_fm(kb, C, st, x[:, 0:tl], x[:, 0:tl], tl, cv[:, 1, ch:ch + 1], cv[:, 2, ch:ch + 1], tmp, psA, psB)
                kb.actf(ob[:, t0:t0 + tl], x[:, 0:tl], AF.Silu)
            kb.dma(kb.sp, C.brT[1][ch], ob)
        kb.barrier()


def phase_conformer_g(kb, C, G, l, st, npc=2):
    if True:
        a = [kb.sbuf(st, "ca%d" % i, [128, NT], F32) for i in range(2)]
        g = [kb.sbuf(st, "cg%d" % i, [128, NT], F32) for i in range(2)]
        pad = [kb.sbuf(st, "cpad%d" % i, [128, NT + 60], BF16) for i in range(2)]
        dg = [kb.sbuf(st, "cdg%d" % i, [128, 31, 128], BF16) for i in range(2)]
        ob = kb.sbuf(st, "cob", [128, NT], BF16)
        tmp = [kb.sbuf(st, "ctmp%d" % i, [128, 512], F32) for i in range(2)]
        lnx = [kb.sbuf(st, "clnx%d" % i, [128, 512], F32) for i in range(2)]
        cw = kb.sbuf(st, "cw", [128, 4, 31], F32)
        cv = kb.sbuf(st, "cv", [128, 3, 4], F32)
        psA = kb.psum(st, "cpsA", [128, 512], F32)
        psB = kb.psum(st, "cpsB", [128, 512], F32)
        pc = [kb.psum(st, "cpc%d" % i, [128, 512], F32) for i in range(npc)]
        kb.dma(kb.sp, cw, G["conf_wT"][l])
        yield
        kb.dma(kb.sp, cv, G["conf_vT"][l])
        yield
        for i in range(2):
            kb.memset(pad[i][:, :], 0.0)
            yield
        poff = [15, 2048 + 45]
        nb = 0
        for ch in range(4):
            a_, g_, pad_, dg_ = a[ch % 2], g[ch % 2], pad[ch % 2], dg[ch % 2]
            kb.dma(kb.sp, a_, C.pT[O_CA // 128 + ch])
            yield
            kb.dma(kb.sp, g_, C.pT[O_CG // 128 + ch])
            yield
            kb.actf(g_[:, :], g_[:, :], AF.Sigmoid)
            yield
            for si, (t0, tl) in enumerate(SEGS):
                kb.tt(pad_[:, poff[si]:poff[si] + tl], a_[:, t0:t0 + tl], g_[:, t0:t0 + tl], ALU.mult)
                yield
            for k in range(31):
                kb.ts(dg_[:, k, :], C.identb[:, :], cw[:, ch, k:k + 1], None, ALU.mult)
                yield
            for (t0, tl) in TBLK:
                si = 0 if t0 < 2048 else 1
                off = poff[si] - 15 + (t0 - SEGS[si][0])
                p = pc[nb % npc]
                x = lnx[nb % 2]
                nb += 1
                for k in range(31):
                    kb.mm(p[:, 0:tl], dg_[:, k, :], pad_[:, off + k:off + k + tl], start=(k == 0), stop=(k == 30))
                    yield
                kb.actf(x[:, 0:tl], p[:, 0:tl], AF.Identity, bias=cv[:, 0, ch:ch + 1])
                yield
                yield from group_ln_fm_g(kb, C, st, x[:, 0:tl], x[:, 0:tl], tl, cv[:, 1, ch:ch + 1], cv[:, 2, ch:ch + 1], tmp, psA, psB)
                kb.actf(ob[:, t0:t0 + tl], x[:, 0:tl], AF.Silu)
                yield
            kb.dma(kb.sp, C.brT[1][ch], ob)
            yield


def gelu_tanh(kb, x, out, t1, t2, n):
    kb.actf(t1, x, AF.Square)
    kb.ts(t1, t1, 0.044715, 1.0, ALU.mult, ALU.add)
    kb.tt(t1, t1, x, ALU.mult)
    kb.actf(t2, t1, AF.Sigmoid, scale=GELU_C)
    kb.tt(out, x, t2, ALU.mult)


def gelu_tanh_g(kb, x, out, t1, t2, n):
    kb.actf(t1, x, AF.Square)
    yield
    kb.ts(t1, t1, 0.044715, 1.0, ALU.mult, ALU.add)
    yield
    kb.tt(t1, t1, x, ALU.mult)
    yield
    kb.actf(t2, t1, AF.Sigmoid, scale=GELU_C)
    yield
    kb.tt(out, x, t2, ALU.mult)
    yield


def phase_sgu(kb, C, G, l):
    with ExitStack() as st:
        u = kb.sbuf(st, "su", [128, NT], F32)
        v = kb.sbuf(st, "sv", [128, NT], F32)
        t1 = kb.sbuf(st, "st1", [128, NT], F32)
        t2 = kb.sbuf(st, "st2", [128, NT], F32)
        ob = kb.sbuf(st, "sob", [128, NT], BF16)
        tmp = [kb.sbuf(st, "stmp%d" % i, [128, 512], F32) for i in range(2)]
        vt = [kb.sbuf(st, "svt%d" % i, [128, 4, 128], BF16) for i in range(2)]
        sv = kb.sbuf(st, "svv", [128, 2, 4], F32)
        ws = kb.sbuf(st, "sws", [128, 4, 128], BF16)
        bsb = kb.sbuf(st, "bsb", [128, 4, 128], F32)
        psA = kb.psum(st, "spsA", [128, 512], F32)
        psB = kb.psum(st, "spsB", [128, 512], F32)
        pst = [kb.psum(st, "spst%d" % i, [128, 512], F32) for i in range(2)]
        pss = [kb.psum(st, "spss%d" % i, [128, 512], F32) for i in range(2)]
        kb.dma(kb.sp, sv, G["sgu_vT"][l])
        kb.dma(kb.pool, ws, G["sgu_wsT"][l].re("g q p -> q g p"))
        kb.dma(kb.sp, bsb[:, :, :].re("p g q -> p (g q)"),
               View(G["sgu_bs"], G["sgu_bs"].ap[l].rearrange("g p -> (g p)").partition_broadcast(128)))
        for g in range(4):
            kb.dma(kb.sp, u, C.pT[O_SU // 128 + g])
            kb.dma(kb.sp, v, C.pT[O_SV // 128 + g])
            gelu_tanh(kb, u[:, :], u[:, :], t1[:, :], t2[:, :], NT)
            gelu_tanh(kb, v[:, :], v[:, :], t1[:, :], t2[:, :], NT)
            for (t0, tl) in TBLK:
                group_ln_fm(kb, C, st, v[:, t0:t0 + tl], v[:, t0:t0 + tl], tl, sv[:, 0, g:g + 1], sv[:, 1, g:g + 1], tmp, psA, psB)
            for bi, (t0, tl) in enumerate(TBLK):
                nt = tl // 128
                pt_, ps_, vv = pst[bi % 2], pss[bi % 2], vt[bi % 2]
                for j in range(nt):
                    kb.mm(pt_[:, j * 128:(j + 1) * 128], v[:, t0 + j * 128:t0 + (j + 1) * 128], C.ident[:, :])
                kb.copy(vv[:, 0:nt, :].re("p a b -> p (a b)"), pt_[:, 0:tl], eng=kb.act)
                for j in range(nt):
                    kb.mm(ps_[:, j * 128:(j + 1) * 128], vv[:, j, :], ws[:, g, :])
                kb.tt(t1[:, t0:t0 + tl].re("p (a b) -> p a b", a=nt), ps_[:, 0:tl].re("p (a b) -> p a b", a=nt),
                      View(bsb, bsb.ap[:, g, :].unsqueeze(1).broadcast_to([128, nt, 128])), ALU.add)
                kb.tt(ob[:, t0:t0 + tl], t1[:, t0:t0 + tl], u[:, t0:t0 + tl], ALU.mult)
            kb.dma(kb.sp, C.brT[3][g], ob)
        kb.barrier()


def phase_sgu_g(kb, C, G, l, st):
    if True:
        u = kb.sbuf(st, "su", [128, NT], F32)
        v = kb.sbuf(st, "sv", [128, NT], F32)
        t1 = kb.sbuf(st, "st1", [128, NT], F32)
        t2 = kb.sbuf(st, "st2", [128, NT], F32)
        ob = kb.sbuf(st, "sob", [128, NT], BF16)
        tmp = [kb.sbuf(st, "stmp%d" % i, [128, 512], F32) for i in range(2)]
        vt = [kb.sbuf(st, "svt%d" % i, [128, 4, 128], BF16) for i in range(2)]
        sv = kb.sbuf(st, "svv", [128, 2, 4], F32)
        ws = kb.sbuf(st, "sws", [128, 4, 128], BF16)
        bsb = kb.sbuf(st, "bsb", [128, 4, 128], F32)
        psA = kb.psum(st, "spsA", [128, 512], F32)
        psB = kb.psum(st, "spsB", [128, 512], F32)
        pst = [kb.psum(st, "spst%d" % i, [128, 512], F32) for i in range(1)] * 2
        pss = [kb.psum(st, "spss%d" % i, [128, 512], F32) for i in range(1)] * 2
        kb.dma(kb.sp, sv, G["sgu_vT"][l])
        yield
        kb.dma(kb.pool, ws, G["sgu_wsT"][l].re("g q p -> q g p"))
        yield
        kb.dma(kb.sp, bsb[:, :, :].re("p g q -> p (g q)"),
               View(G["sgu_bs"], G["sgu_bs"].ap[l].rearrange("g p -> (g p)").partition_broadcast(128)))
        yield
        for g in range(4):
            kb.dma(kb.sp, u, C.pT[O_SU // 128 + g])
            yield
            kb.dma(kb.sp, v, C.pT[O_SV // 128 + g])
            yield
            yield from gelu_tanh_g(kb, u[:, :], u[:, :], t1[:, :], t2[:, :], NT)
            yield from gelu_tanh_g(kb, v[:, :], v[:, :], t1[:, :], t2[:, :], NT)
            for (t0, tl) in TBLK:
                yield from group_ln_fm_g(kb, C, st, v[:, t0:t0 + tl], v[:, t0:t0 + tl], tl, sv[:, 0, g:g + 1], sv[:, 1, g:g + 1], tmp, psA, psB)
            for bi, (t0, tl) in enumerate(TBLK):
                nt = tl // 128
                pt_, ps_, vv = pst[bi % 2], pss[bi % 2], vt[bi % 2]
                for j in range(nt):
                    kb.mm(pt_[:, j * 128:(j + 1) * 128], v[:, t0 + j * 128:t0 + (j + 1) * 128], C.ident[:, :])
                    yield
                kb.copy(vv[:, 0:nt, :].re("p a b -> p (a b)"), pt_[:, 0:tl], eng=kb.act)
                yield
                for j in range(nt):
                    kb.mm(ps_[:, j * 128:(j + 1) * 128], vv[:, j, :], ws[:, g, :])
                    yield
                kb.tt(t1[:, t0:t0 + tl].re("p (a b) -> p a b", a=nt), ps_[:, 0:tl].re("p (a b) -> p a b", a=nt),
                      View(bsb, bsb.ap[:, g, :].unsqueeze(1).broadcast_to([128, nt, 128])), ALU.add)
                yield
                kb.tt(ob[:, t0:t0 + tl], t1[:, t0:t0 + tl], u[:, t0:t0 + tl], ALU.mult)
                yield
            kb.dma(kb.sp, C.brT[3][g], ob)
            yield


def phase_fft(kb, C, G, l):
    with ExitStack() as st:
        hT = kb.sbuf(st, "fh", [128, 4, NT], BF16)
        cs = kb.sbuf(st, "fcs", [128, 256], BF16)
        AB = kb.sbuf(st, "fab", [128, NTILE, 4, 256], BF16)
        tab = [kb.sbuf(st, "ftab%d" % i, [128, 2, 16, 512], BF16) for i in range(2)]
        ob = kb.sbuf(st, "fob", [128, 4, NT], BF16)
        pa = [kb.psum(st, "fpa%d" % i, [128, 256], F32) for i in range(2)]
        py = [kb.psum(st, "fpy%d" % i, [128, 512], F32) for i in range(2)]
        kb.dma(kb.pool, cs, G["dft_c"])
        for g in range(4):
            kb.dma(kb.pool, hT[:, g, :], C.pT[O_F // 128 + g])
        n = 0
        for t in range(NTILE):
            for g in range(4):
                p = pa[n % 2]
                kb.mm(p[:, :], hT[:, g, t * 128:(t + 1) * 128], cs[:, :])
                kb.copy(AB[:, t, g, :], p[:, :], eng=kb.act if n % 2 == 0 else kb.dve)
                n += 1
        n = 0
        for kbk in range(4):
            tb = tab[kbk % 2]
            for cs_i in range(2):
                kb.dma(kb.pool, tb[:, cs_i, :, :], G["dft_lat"][cs_i].re("(t p) k -> p t k", p=128)[:, :, kbk * 512:(kbk + 1) * 512])
            for g in range(4):
                p = py[n % 2]
                for t in range(16):
                    kb.mm(p[:, :], AB[:, t, g, 0:128], tb[:, 0, t, :], start=(t == 0), stop=False)
                    kb.mm(p[:, :], AB[:, t, g, 128:256], tb[:, 1, t, :], start=False, stop=(t == 15))
                kb.copy(ob[:, g, kbk * 512:(kbk + 1) * 512], p[:, :], eng=kb.act if n % 2 == 0 else kb.dve)
                n += 1
        tb = tab[0]
        for cs_i in range(2):
            kb.dma(kb.pool, tb[:, cs_i, 0:2, 0:256], G["dft_ctx"][cs_i].re("(t p) k -> p t k", p=128))
        for g in range(4):
            p = py[n % 2]
            for t in range(2):
                kb.mm(p[:, 0:256], AB[:, 16 + t, g, 0:128], tb[:, 0, t, 0:256], start=(t == 0), stop=False)
                kb.mm(p[:, 0:256], AB[:, 16 + t, g, 128:256], tb[:, 1, t, 0:256], start=False, stop=(t == 1))
            kb.copy(ob[:, g, 2048:2304], p[:, 0:256], eng=kb.act if n % 2 == 0 else kb.dve)
            n += 1
        for g in range(4):
            kb.dma(kb.sp, C.brT[2][g], ob[:, g, :])
        kb.barrier()


def phase_merge(kb, C, G, l, last=False):
    blks = TBLK[:4] if last else TBLK
    ntl = 16 if last else NTILE
    with ExitStack() as st0:
      wo = kb.sbuf(st0, "mwo", [128, 8, D], BF16)
      mT = kb.sbuf(st0, "mT", [128, 8, NT], BF16)
      with ExitStack() as st:
        br = kb.sbuf(st, "mbr", [128, 16, NT], BF16)
        wb = kb.sbuf(st, "mwb", [128, 16, D], BF16)
        gt = [kb.sbuf(st, "mgt%d" % i, [128, NT], F32) for i in range(2)]
        macc = kb.sbuf(st, "macc", [128, NT], F32)
        mtmp = [kb.sbuf(st, "mtmp%d" % i, [128, 512], F32) for i in range(2)]
        pp = [kb.psum(st, "mpp%d" % i, [128, 512], F32) for i in range(2)]
        for i in range(4):
            for kc in range(4):
                kb.dma(kb.sp, br[:, i * 4 + kc, :], C.brT[i][kc])
            kb.dma(kb.pool, wb[:, i * 4:(i + 1) * 4, :], G["w_branch"][l, i].re("(k p) d -> p k d", p=128))
        kb.dma(kb.pool, wo, G["w_out"][l].re("(k p) d -> p k d", p=128))
        n = 0
        for dc in range(8):
            for i in range(4):
                g = gt[n % 2]
                n += 1
                kb.dma(kb.sp, g, C.pT[O_G // 128 + i * 8 + dc])
                kb.actf(g[:, :], g[:, :], AF.Sigmoid)
                for bi, (t0, tl) in enumerate(blks):
                    p = pp[bi % 2]
                    for kc in range(4):
                        kb.mm(p[:, 0:tl], wb[:, i * 4 + kc, dc * 128:(dc + 1) * 128], br[:, i * 4 + kc, t0:t0 + tl],
                              start=(kc == 0), stop=(kc == 3))
                    if i == 0:
                        kb.tt(macc[:, t0:t0 + tl], p[:, 0:tl], g[:, t0:t0 + tl], ALU.mult)
                    else:
                        tm = mtmp[bi % 2]
                        kb.tt(tm[:, 0:tl], p[:, 0:tl], g[:, t0:t0 + tl], ALU.mult)
                        kb.tt(macc[:, t0:t0 + tl], macc[:, t0:t0 + tl], tm[:, 0:tl], ALU.add)
            kb.copy(mT[:, dc, :], macc[:, :], eng=kb.act)
        kb.barrier()
      if True:
        with ExitStack() as st2:
            g1 = [kb.sbuf(st2, "g1_%d" % i, [128, D], F32) for i in range(2)]
            bo = kb.sbuf(st2, "bo", [128, D], F32)
            lg = kb.sbuf(st2, "lg1", [128, D], F32)
            lb = kb.sbuf(st2, "lb1", [128, D], F32)
            dg = kb.sbuf(st2, "dg", [128, 128], F32)
            psbc = kb.psum(st2, "psbc", [128, 1024], F32)
            for seg in range(2):
                bcast_from_modT(kb, dg, psbc, C, 16, seg, g1[seg][:, :])
            for nm, dst in (("b_out", bo), ("ln1_g", lg), ("ln1_b", lb)):
                kb.dma(kb.sp, dst, View(G[nm], G[nm].ap[l].partition_broadcast(128)))
            residual_ln(kb, C, st2, lambda t, dh, p: [kb.mm(p[:, :], mT[:, k, t * 128:(t + 1) * 128], wo[:, k, dh * 512:(dh + 1) * 512],
                                                          start=(k == 0), stop=(k == 7)) for k in range(8)],
                        g1, bo, lg, lb, None, ntile=ntl)
            kb.barrier()


def residual_ln(kb, C, st, emit_mm, gbc, bias_bc, lng, lnb, acc, out_final=None, ntile=NTILE):
    ht = [kb.sbuf(st, "rh%d" % i, [128, D], F32) for i in range(2)]
    yt = [kb.sbuf(st, "ry%d" % i, [128, D], F32) for i in range(2)]
    junk = [kb.sbuf(st, "rjunk%d" % i, [128, D], F32) for i in range(2)]
    sm = [kb.sbuf(st, "rsm%d" % i, [128, 4], F32) for i in range(2)]
    pp = [kb.psum(st, "rpp%d" % i, [128, 512], F32) for i in range(4)] if emit_mm is not None else None

    def tile_g(t):
        seg = seg_of_tile(t)
        h, y, s_ = ht[t % 2], yt[t % 2], sm[t % 2]
        kb.dma(kb.sp, h, C.h[t])
        yield
        if emit_mm is not None:
            for dh in range(2):
                p = pp[(t * 2 + dh) % 4]
                emit_mm(t, dh, p)
                kb.tt(y[:, dh * 512:(dh + 1) * 512], p[:, :], bias_bc[:, dh * 512:(dh + 1) * 512], ALU.add)
                yield
            kb.tt(y[:, :], y[:, :], gbc[seg][:, :], ALU.mult)
        else:
            kb.tt(y[:, :], acc[t][:, :], gbc[seg][:, :], ALU.mult)
        yield
        kb.stt(h[:, :], h[:, :], ALPHA, y[:, :], ALU.mult, ALU.add)
        yield
        yield from ln_stats_g(kb, h[:, :], y[:, :], junk[t % 2][:, :], s_)
        kb.actf(y[:, :], y[:, :], AF.Identity, scale=s_[:, 3:4])
        yield
        kb.tt(y[:, :], y[:, :], lng[:, :], ALU.mult)
        yield
        kb.tt(y[:, :], y[:, :], lnb[:, :], ALU.add)
        yield
        if out_final is not None:
            if t < 16:
                kb.dma(kb.sp, out_final[t * 128:(t + 1) * 128, :], y)
        else:
            kb.dma(kb.sp, C.h[t], y)
    for t0 in range(0, ntile, 2):
        lockstep([tile_g(t) for t in range(t0, min(t0 + 2, ntile))])


def phase_moe(kb, C, G, l, last):
    with ExitStack() as st:
        xT = kb.sbuf(st, "xT2", [128, 8, NT], BF16)
        comb = kb.sbuf(st, "comb", [128, NTILE, 16], F32)
        ntl = 16 if last else NTILE
        blks = TBLK[:4] if last else TBLK
        phase_lnT(kb, C, G, l, 1, xT, router=comb, ntile=ntl)
        acc = [kb.sbuf(st, "acc%d" % t, [128, D], F32) for t in range(NTILE)]
        with ExitStack() as st1:
            wg = [kb.sbuf(st1, "wg%d" % i, [128, 8, 512], BF16) for i in range(2)]
            wu = [kb.sbuf(st1, "wu%d" % i, [128, 8, 512], BF16) for i in range(2)]
            wd = [kb.sbuf(st1, "wd%d" % i, [128, 4, D], BF16) for i in range(2)]
            hid = [kb.sbuf(st1, "hid%d" % i, [128, 4, 512], BF16) for i in range(2)]
            sg = [kb.sbuf(st1, "sg%d" % i, [128, 512], F32) for i in range(2)]
            pg = [kb.psum(st1, "pg%d" % i, [128, 512], F32) for i in range(2)]
            pu = [kb.psum(st1, "pu%d" % i, [128, 512], F32) for i in range(2)]
            po = [kb.psum(st1, "po%d" % i, [128, 512], F32) for i in range(4)]
            n = 0
            m = 0
            for e in range(16):
                g_, u_, d_ = wg[e % 2], wu[e % 2], wd[e % 2]
                kb.dma(kb.pool, g_, G["w_gate"][l, e].re("(k p) h -> p k h", p=128))
                kb.dma(kb.pool, u_, G["w_up"][l, e].re("(k p) h -> p k h", p=128))
                kb.dma(kb.pool, d_, G["w_down"][l, e].re("(k p) d -> p k d", p=128))
                for bi, (t0, tl) in enumerate(blks):
                    hd = hid[bi % 2]
                    for hc in range(4):
                        a, b, s_ = pg[n % 2], pu[n % 2], sg[n % 2]
                        n += 1
                        for k in range(8):
                            kb.mm(a[:, 0:tl], g_[:, k, hc * 128:(hc + 1) * 128], xT[:, k, t0:t0 + tl], start=(k == 0), stop=(k == 7))
                        for k in range(8):
                            kb.mm(b[:, 0:tl], u_[:, k, hc * 128:(hc + 1) * 128], xT[:, k, t0:t0 + tl], start=(k == 0), stop=(k == 7))
                        kb.actf(s_[:, 0:tl], a[:, 0:tl], AF.Silu)
                        kb.tt(hd[:, hc, 0:tl], s_[:, 0:tl], b[:, 0:tl], ALU.mult)
                    for tt_ in range(tl // 128):
                        t = t0 // 128 + tt_
                        for dh in range(2):
                            p = po[m % 4]
                            m += 1
                            for hc in range(4):
                                kb.mm(p[:, :], hd[:, hc, tt_ * 128:(tt_ + 1) * 128], d_[:, hc, dh * 512:(dh + 1) * 512],
                                      start=(hc == 0), stop=(hc == 3))
                            dst = acc[t][:, dh * 512:(dh + 1) * 512]
                            if e == 0:
                                kb.ts(dst, p[:, :], comb[:, t, e:e + 1], None, ALU.mult)
                            else:
                                kb.stt(dst, p[:, :], comb[:, t, e:e + 1], dst, ALU.mult, ALU.add)
            kb.barrier()
        with ExitStack() as st2:
            g2 = [kb.sbuf(st2, "g2_%d" % i, [128, D], F32) for i in range(2)]
            lg = kb.sbuf(st2, "lg2", [128, D], F32)
            lb = kb.sbuf(st2, "lb2", [128, D], F32)
            dg = kb.sbuf(st2, "dg", [128, 128], F32)
            psbc = kb.psum(st2, "psbc", [128, 1024], F32)
            for seg in range(2):
                bcast_from_modT(kb, dg, psbc, C, 40, seg, g2[seg][:, :])
            for nm, dst in (("ln2_g", lg), ("ln2_b", lb)):
                kb.dma(kb.sp, dst, View(G[nm], G[nm].ap[l].partition_broadcast(128)))
            residual_ln(kb, C, st2, None, g2, None, lg, lb, acc, out_final=(G["out"] if last else None), ntile=ntl)
            kb.barrier()


def phase_conf_sgu(kb, C, G, l):
    with ExitStack() as st:
        lockstep([phase_conformer_g(kb, C, G, l, st), phase_sgu_g(kb, C, G, l, st)])
        kb.barrier()


def phase_inproj_g(kb, C, G, l, xT, st, prog):
    wt = [kb.sbuf(st, "wi%d" % i, [128, 8, 128], BF16) for i in range(3)]
    stg = [kb.sbuf(st, "stg%d" % i, [128, NT], F32) for i in range(2)]
    bi = kb.sbuf(st, "bi", [128, NCH], F32)
    pp = [kb.psum(st, "pp%d" % i, [128, 512], F32) for i in range(2)]
    L = lnT_alloc(kb, st, C, G, l, 0, None, npst=2)
    kb.dma(kb.sp, bi, G["b_inT"][l])
    n = 0
    order = [68] + list(range(68))
    for ci, ch in enumerate(order):
        w = wt[ci % 3]
        kb.dma(kb.pool, w, G["w_inR"][l, ch])
        rows = 128 if ch < 68 else 16
        sg = stg[ci % 2]
        for (t0, tl) in TBLK:
            if ci == 0:
                lnT_tiles(kb, C, L, range(t0 // 128, (t0 + tl) // 128), xT)
            p = pp[n % 2]
            n += 1
            for k in range(8):
                kb.mm(p[0:rows, 0:tl], w[:, k, 0:rows], xT[:, k, t0:t0 + tl], start=(k == 0), stop=(k == 7))
            kb.actf(sg[0:rows, t0:t0 + tl], p[0:rows, 0:tl], AF.Identity, bias=bi[0:rows, ch:ch + 1])
            yield
        kb.dma(kb.sp, C.pT[ch][0:rows, :], sg[0:rows, :])
        prog["ch"] = ci + 1
        yield


def phase_inproj_gdnA(kb, C, G, l, GD):
    with ExitStack() as st:
        xT = kb.sbuf(st, "xT1", [128, 8, NT], BF16)
        prog = {"ch": 0}
        gi = phase_inproj_g(kb, C, G, l, xT, st, prog)
        while prog["ch"] < 13:
            next(gi)
        ga = gdn_stepA_g(kb, C, G, l, GD, st)
        done_i = done_a = False
        while not (done_i and done_a):
            if not done_i:
                try:
                    next(gi)
                except StopIteration:
                    done_i = True
            for _ in range(3):
                if not done_a:
                    try:
                        next(ga)
                    except StopIteration:
                        done_a = True
        kb.barrier()


def phase_mod_g(kb, C, G, l, st):
    modT, onep = C.modT_all[l], C.onep_all[l]
    cl = kb.sbuf(st, "cl", [128, 8], F32)
    cc = kb.sbuf(st, "cc", [128, 8], F32)
    sc = kb.sbuf(st, "sc", [128, 8, 2], F32)
    bm = kb.sbuf(st, "bm", [128, 48], F32)
    wm = [kb.sbuf(st, "wm%d" % i, [128, 8, 768], F32) for i in range(2)]
    ps = kb.psum(st, "psmod", [128, 48, 2], F32)
    kb.dma(kb.sp, cl, G["cT"])
    kb.dma(kb.sp, cc, G["cctxT"])
    kb.dma(kb.sp, bm, G["b_modT"][l])
    kb.actf(sc[:, :, 0], cl[:, :], AF.Silu)
    kb.actf(sc[:, :, 1], cc[:, :], AF.Silu)
    yield
    wsrc = G["w_mod"][l].re("(k p) n -> p k n", p=128)
    for ng in range(8):
        w = wm[ng % 2]
        kb.dma(kb.sp, w, wsrc[:, :, ng * 768:(ng + 1) * 768])
        yield
        for j in range(6):
            ch = ng * 6 + j
            for k in range(8):
                kb.mm(ps[:, ch, :], w[:, k, j * 128:(j + 1) * 128], sc[:, k, :], start=(k == 0), stop=(k == 7))
            yield
    kb.tt(modT[:, :, :], ps[:, :, :], View(bm, bm.ap.unsqueeze(2).broadcast_to([128, 48, 2])), ALU.add)
    yield
    kb.ts(onep[:, 0:8, :], modT[:, 8:16, :], 1.0, None, ALU.add)
    kb.ts(onep[:, 8:16, :], modT[:, 32:40, :], 1.0, None, ALU.add)
    yield


def phase_h0_g(kb, C, G, st):
    xt = [kb.sbuf(st, "xt%d" % i, [128, D], F32) for i in range(2)]
    pt = [kb.sbuf(st, "pt%d" % i, [128, D], F32) for i in range(2)]
    for t in range(NTILE):
        a = xt[t % 2]
        if t < 16:
            b_ = pt[t % 2]
            kb.dma(kb.sp, a, G["x"][t * 128:(t + 1) * 128, :])
            kb.dma(kb.sp, b_, G["pos"][t * 128:(t + 1) * 128, :])
            kb.tt(a[:, :], a[:, :], b_[:, :], ALU.add)
        else:
            kb.dma(kb.sp, a, G["ctx"][(t - 16) * 128:(t - 15) * 128, :])
        kb.dma(kb.sp, C.h[t], a)
        yield


def phase_h0_mod0(kb, C, G):
    with ExitStack() as st:
        lockstep([phase_h0_g(kb, C, G, st), phase_mod_g(kb, C, G, 0, st)])
        kb.barrier()


def phase_conf_sgu_mod(kb, C, G, l, lnext):
    with ExitStack() as st:
        gens = [phase_conformer_g(kb, C, G, l, st, npc=1), phase_sgu_g(kb, C, G, l, st)]
        gm = phase_mod_g(kb, C, G, lnext, st)
        r = 0
        mod_live = True
        while gens or mod_live:
            for g_ in list(gens):
                try:
                    next(g_)
                except StopIteration:
                    gens.remove(g_)
            r += 1
            if mod_live and (r % 15 == 0 or not gens):
                try:
                    next(gm)
                except StopIteration:
                    mod_live = False
        kb.barrier()


def chunk_off(n):
    return n * 64


ORDER_F = [32, 33, 34, 35] + list(range(32))
ORDER_B = [35, 34, 33, 32] + list(range(31, -1, -1))


def bc3(v, shape):
    return View(v.buf, v.ap.broadcast_to(shape))


def gdn_alloc(kb, st0, C, G):
    GD = Ctx()
    GD.qT = kb.sbuf(st0, "qT", [128, 4, NT], BF16)
    GD.kT = kb.sbuf(st0, "kT", [128, 4, NT], BF16)
    GD.vT = kb.sbuf(st0, "vT", [128, 4, NT], BF16)
    GD.gb = kb.sbuf(st0, "gb", [128, 36, 8], F32)
    GD.gm2 = kb.sbuf(st0, "gm2", [128, 3, 64], F32)
    kb.dma(kb.sp, GD.gm2, G["gm2"])
    GD.gml = kb.sbuf(st0, "gml", [128, 5, 128], F32)
    kb.dma(kb.sp, GD.gml, G["gml"])
    return GD


def gdn_stepA_g(kb, C, G, l, GD, st):
    qT, kT, vT, gb = GD.qT, GD.kT, GD.vT, GD.gb
    pad = [kb.sbuf(st, "gpad%d" % i, [128, NT + 8], BF16) for i in range(2)]
    dgc = [kb.sbuf(st, "gdg%d" % i, [128, 5, 128], BF16) for i in range(2)]
    acc = [[kb.sbuf(st, "gacc%d%d" % (a, i), [128, 512], F32) for i in range(2)] for a in range(2)]
    sq = [[kb.sbuf(st, "gsq%d%d" % (a, i), [128, 512], F32) for i in range(2)] for a in range(2)]
    rs = [[kb.sbuf(st, "grs%d%d" % (a, i), [128, 512], F32) for i in range(2)] for a in range(2)]
    cw = kb.sbuf(st, "gcw", [128, 12, 5], F32)
    ab8 = kb.sbuf(st, "ab8", [40, NT], F32)
    ab = kb.sbuf(st, "gab", [8, 4], F32)
    pcv2 = [kb.psum(st, "gpcv%d" % i, [128, 512], F32) for i in range(2)]
    pss2 = [kb.psum(st, "gpss%d" % i, [128, 512], F32) for i in range(2)]
    psg = pss2[0][:, 0:288].re("p (a b) -> p a b", a=36)
    kb.dma(kb.sp, cw, G["gdn_cwT"][l])
    kb.dma(kb.sp, ab[:, 0:2], G["gdn_ab"][l])
    for i in range(2):
        kb.memset(pad[i][:, :], 0.0)
    yield
    poff = [2, 2048 + 6]

    def chunk_g(j):
        par = j % 2
        pd, dg, pcv, pss = pad[par], dgc[par], pcv2[par], pss2[par]
        for si, (t0, tl) in enumerate(SEGS):
            kb.dma(kb.pool, pd[:, poff[si]:poff[si] + tl], C.pT[j][:, t0:t0 + tl])
        yield
        for k in range(5):
            kb.ts(dg[:, k, :], C.identb[:, :], cw[:, j, k:k + 1], None, ALU.mult)
        yield
        dst = (qT, kT, vT)[j // 4]
        h = j % 4
        nb = 0
        for (t0, tl) in TBLK:
            si = 0 if t0 < 2048 else 1
            off = poff[si] - 2 + (t0 - SEGS[si][0])
            for k in range(5):
                kb.mm(pcv[:, 0:tl], dg[:, k, :], pd[:, off + k:off + k + tl], start=(k == 0), stop=(k == 4))
            yield
            if j >= 8:
                kb.actf(dst[:, h, t0:t0 + tl], pcv[:, 0:tl], AF.Silu)
                yield
            else:
                x, q_, r_ = acc[par][nb % 2], sq[par][nb % 2], rs[par][nb % 2]
                nb += 1
                kb.actf(x[:, 0:tl], pcv[:, 0:tl], AF.Silu)
                yield
                kb.tt(q_[:, 0:tl], x[:, 0:tl], x[:, 0:tl], ALU.mult)
                yield
                kb.mm(pss[:, 0:tl], C.ones[:, :], q_[:, 0:tl])
                yield
                kb.ts(r_[:, 0:tl], pss[:, 0:tl], EPS, None, ALU.add)
                yield
                kb.actf(r_[:, 0:tl], r_[:, 0:tl], AF.Sqrt)
                yield
                kb.recip(r_[:, 0:tl], r_[:, 0:tl])
                yield
                if j < 4:
                    kb.stt(dst[:, h, t0:t0 + tl], x[:, 0:tl], 128.0 ** -0.5, r_[:, 0:tl], ALU.mult, ALU.mult)
                else:
                    kb.tt(dst[:, h, t0:t0 + tl], x[:, 0:tl], r_[:, 0:tl], ALU.mult)
                yield

    for j0 in range(0, 12, 2):
        gens = [chunk_g(j0), chunk_g(j0 + 1)]
        while gens:
            for g_ in list(gens):
                try:
                    next(g_)
                except StopIteration:
                    gens.remove(g_)
            yield
    a8, b8 = ab8[0:8, :], ab8[32:40, :]
    kb.dma(kb.sp, a8, C.pT[68][0:8, :])
    kb.dma(kb.sp, b8, C.pT[68][8:16, :])
    yield
    kb.actf(a8, a8, AF.Exp, bias=ab[:, 1:2])
    yield
    kb.ts(a8, a8, 1.0, None, ALU.add)
    yield
    kb.actf(a8, a8, AF.Ln)
    yield
    kb.actf(ab[:, 2:3], ab[:, 0:1], AF.Exp)
    kb.ts(ab[:, 3:4], ab[:, 2:3], -1.0, None, ALU.mult)
    yield
    kb.ts(a8, a8, ab[:, 3:4], None, ALU.mult)
    yield
    kb.actf(b8, b8, AF.Sigmoid)
    yield
    idb_ = C.ident[32:40, 32:40]
    for n in range(36):
        sl = slice(n * 64, n * 64 + 64)
        sf, sb = ORDER_F.index(n), ORDER_B.index(n)
        kb.mm(psg[0:64, sf, 0:4], ab8[0:8, sl], C.ident[0:8, 0:4])
        kb.mm(psg[64:128, sb, 0:4], ab8[0:8, sl], C.ident[0:8, 4:8])
        kb.mm(psg[0:64, sf, 4:8], ab8[32:40, sl], C.ident[32:40, 32:36])
        kb.mm(psg[64:128, sb, 4:8], ab8[32:40, sl], C.ident[32:40, 36:40])
        if n % 4 == 3:
            yield
    kb.copy(gb[:, :, :], psg[:, :, :])
    yield


def gdn_stepBC(kb, C, G, l, GD):
    qT, kT, vT, gb, gm2, gml = GD.qT, GD.kT, GD.vT, GD.gb, GD.gm2, GD.gml
    if True:
        with ExitStack() as st:
            oT = kb.sbuf(st, "oT", [128, 4, NT], BF16)
            S = kb.sbuf(st, "S", [128, 8, 128], F32)
            Sbf = kb.sbuf(st, "Sbf", [128, 8, 128], BF16)
            X = [kb.psum(st, "gX%d" % i, [128, 512], F32) for i in range(3)]
            Yp = [kb.psum(st, "gY%d" % i, [128, 512], F32) for i in range(3)]
            XS = kb.psum(st, "gXS", [128, 1024], F32)
            visited = set()
            stw = ExitStack()
            w = {}
            for name, shape, dt in [("ktok", [128, 4, 128], F32), ("vb", [128, 4, 128], F32), ("kbg", [128, 4, 128], F32),
                                    ("u", [128, 4, 128], F32), ("kd", [128, 4, 128], BF16), ("vn", [128, 4, 128], BF16),
                                    ("obf", [128, 4, 128], BF16), ("sm", [128, 64], F32),
                                    ("G0", [128, 4, 64], F32), ("G1", [128, 4, 64], F32), ("E", [128, 4, 64], F32),
                                    ("ET", [128, 4, 64], F32), ("P", [128, 4, 64], F32), ("Q", [128, 4, 64], F32),
                                    ("Y", [128, 4, 64], F32), ("P2", [128, 4, 64], F32), ("Q2", [128, 4, 64], F32),
                                    ("Y2", [128, 4, 64], F32), ("at", [128, 4, 64], BF16), ("wT", [128, 8, 64], BF16)]:
                w[name] = kb.sbuf(stw, "g" + name, shape, dt)
            kb.memset(S[:, :, :], 0.0, eng=kb.pool)
            kb.memset(Sbf[:, :, :], 0.0, eng=kb.pool)
            HALF = (slice(0, 64), slice(64, 128))
            idf = [C.ident[0:64, 0:64], C.ident[64:128, 64:128]]
            idb = [C.identb[0:64, 0:64], C.identb[64:128, 64:128]]

            def mbc(k):
                return View(gm2, gm2.ap[:, k, :].unsqueeze(1).broadcast_to([128, 4, 64]))
            maskA, maskB, ident4 = mbc(0), mbc(1), mbc(2)

            def fl(v):
                return v.re("p h c -> p (h c)")
            for s in range(36):
                nch = (ORDER_F[s], ORDER_B[s])
                sls = [slice(n * 64, n * 64 + 64) for n in nch]
                g4 = gb[:, s, 0:4]
                beta4 = gb[:, s, 4:8]
                sm = w["sm"]
                egc, ekd, bg, nb_, gcs, egl8 = (sm[:, 0:4], sm[:, 4:8], sm[:, 8:12], sm[:, 12:16], sm[:, 16:20], sm[:, 24:32])
                for h in range(4):
                    for d in range(2):
                        kb.mm(X[0][HALF[d], h * 128:(h + 1) * 128], kT[:, h, sls[d]], C.identb[:, :])
                        kb.mm(X[1][HALF[d], h * 128:(h + 1) * 128], vT[:, h, sls[d]], C.identb[:, :])
                kb.mm(Yp[0][:, 0:4], gml[:, 0, :], g4)
                kb.mm(Yp[0][:, 4:8], gml[:, 2, :], g4)
                kb.mm(Yp[0][:, 8:12], gml[:, 3, :], g4)
                kb.mm(Yp[0][:, 12:16], gml[:, 4, :], g4)
                kb.actf(egc, Yp[0][:, 0:4], AF.Exp)
                kb.actf(egl8, Yp[0][:, 8:16], AF.Exp)
                kb.copy(gcs, Yp[0][:, 0:4], eng=kb.act)
                kb.tt(ekd, Yp[0][:, 4:8], gcs, ALU.subtract)
                kb.actf(ekd, ekd, AF.Exp)
                kb.tt(bg, beta4, egc, ALU.mult)
                kb.ts(nb_, beta4, -1.0, None, ALU.mult)
                k3 = X[0][:, :].re("p (h c) -> p h c", h=4)
                v3 = X[1][:, :].re("p (h c) -> p h c", h=4)
                kb.copy(w["ktok"][:, :, :], k3, eng=kb.act)
                kb.tt(w["vb"][:, :, :], v3, bc3(View(gb, beta4.ap.unsqueeze(2)), [128, 4, 128]), ALU.mult)
                kb.tt(w["kbg"][:, :, :], w["ktok"][:, :, :], bc3(View(sm, bg.ap.unsqueeze(2)), [128, 4, 128]), ALU.mult)
                kb.tt(w["kd"][:, :, :], w["ktok"][:, :, :], bc3(View(sm, ekd.ap.unsqueeze(2)), [128, 4, 128]), ALU.mult, eng=kb.pool)
                gbc = bc3(View(gb, g4.ap.unsqueeze(2)), [128, 4, 64])
                kb.tt(w["G1"][:, :, :], maskA, gbc, ALU.mult)
                kb.tt(w["G0"][:, :, :], maskB, gbc, ALU.mult, eng=kb.pool)
                kb.mm(X[2][:, 0:256], gml[:, 0, :], fl(w["G1"][:, :, :]))
                kb.mm(X[2][:, 256:512], gml[:, 1, :], fl(w["G0"][:, :, :]))
                kb.actf(fl(w["E"][:, :, :]), X[2][:, 0:256], AF.Exp)
                kb.actf(fl(w["ET"][:, :, :]), X[2][:, 256:512], AF.Exp)
                kb.tt(w["E"][:, :, :], w["E"][:, :, :], maskA, ALU.mult)
                kb.tt(w["ET"][:, :, :], w["ET"][:, :, :], maskB, ALU.mult, eng=kb.pool)
                for h in range(4):
                    for d in range(2):
                        kb.mm(Yp[1][HALF[d], h * 64:(h + 1) * 64], kT[:, h, sls[d]], kT[:, h, sls[d]])
                        kb.mm(Yp[0][HALF[d], 256 + h * 64:256 + (h + 1) * 64], kT[:, h, sls[d]], qT[:, h, sls[d]])
                kk3 = Yp[1][:, 0:256].re("p (h c) -> p h c", h=4)
                qk3 = Yp[0][:, 256:512].re("p (h c) -> p h c", h=4)
                kb.tt(w["E"][:, :, :], kk3, w["E"][:, :, :], ALU.mult)
                kb.tt(w["P"][:, :, :], w["E"][:, :, :], bc3(View(sm, nb_.ap.unsqueeze(2)), [128, 4, 64]), ALU.mult)
                for h in range(4):
                    for d in range(2):
                        kb.mm(Yp[1][HALF[d], 256 + h * 64:256 + (h + 1) * 64], w["P"][HALF[d], h, :], idf[d])
                kb.copy(fl(w["Q"][:, :, :]), Yp[1][:, 256:512], eng=kb.act)
                kb.tt(w["Y"][:, :, :], w["Q"][:, :, :], ident4, ALU.add)
                P, Q, Y = w["P"], w["Q"], w["Y"]
                P2, Q2, Y2 = w["P2"], w["Q2"], w["Y2"]
                pendZ = None
                for lev in range(1, 6):
                    for h in range(4):
                        for d in range(2):
                            kb.mm(X[0][HALF[d], h * 64:(h + 1) * 64], Q[HALF[d], h, :], P[HALF[d], h, :])
                    if lev < 5:
                        for h in range(4):
                            for d in range(2):
                                kb.mm(Yp[1][HALF[d], h * 64:(h + 1) * 64], P[HALF[d], h, :], Q[HALF[d], h, :])
                    if pendZ is not None:
                        Pz, Ya, Yb = pendZ
                        for h in range(4):
                            for d in range(2):
                                kb.mm(X[1][HALF[d], h * 64:(h + 1) * 64], Pz[HALF[d], h, :], Ya[HALF[d], h, :])
                    kb.copy(fl(P2[:, :, :]), X[0][:, 0:256], eng=kb.act)
                    if lev < 5:
                        kb.copy(fl(Q2[:, :, :]), Yp[1][:, 0:256])
                    if pendZ is not None:
                        kb.tt(fl(Yb[:, :, :]), fl(Ya[:, :, :]), X[1][:, 0:256], ALU.add)
                    pendZ = (P2, Y, Y2)
                    P, P2 = P2, P
                    Q, Q2 = Q2, Q
                    Y, Y2 = Y2, Y
                Pz, Ya, Yb = pendZ
                for h in range(4):
                    for d in range(2):
                        kb.mm(X[1][HALF[d], h * 64:(h + 1) * 64], Pz[HALF[d], h, :], Ya[HALF[d], h, :])
                kb.tt(fl(Yb[:, :, :]), fl(Ya[:, :, :]), X[1][:, 0:256], ALU.add)
                kb.tt(w["at"][:, :, :], qk3, w["ET"][:, :, :], ALU.mult)
                for h in range(4):
                    for d in range(2):
                        kb.mm(X[2][HALF[d], h * 128:(h + 1) * 128], Y[HALF[d], h, :], w["vb"][HALF[d], h, :])
                        kb.mm((Yp[0], Yp[2])[d][:, h * 64:(h + 1) * 64], w["kbg"][HALF[d], h, :], Y[HALF[d], h, :])
                kb.copy(fl(w["u"][:, :, :]), X[2][:, :], eng=kb.act)
                kb.copy(fl(w["wT"][:, 0:4, :]), Yp[0][:, 0:256])
                kb.copy(fl(w["wT"][:, 4:8, :]), Yp[2][:, 0:256])
                for h in range(4):
                    for d in range(2):
                        kb.mm(X[0][HALF[d], h * 128:(h + 1) * 128], w["wT"][:, d * 4 + h, :], Sbf[:, d * 4 + h, :])
                        kb.mm(X[1][HALF[d], h * 128:(h + 1) * 128], qT[:, h, sls[d]], Sbf[:, d * 4 + h, :])
                kb.tt(fl(w["vn"][:, :, :]), fl(w["u"][:, :, :]), X[0][:, :], ALU.subtract)
                for h in range(4):
                    for d in range(2):
                        kb.mm(X[2][HALF[d], h * 128:(h + 1) * 128], w["at"][HALF[d], h, :], w["vn"][HALF[d], h, :])
                for h in range(4):
                    for d in range(2):
                        kb.mm(XS[:, (d * 4 + h) * 128:(d * 4 + h + 1) * 128], w["kd"][HALF[d], h, :], w["vn"][HALF[d], h, :])
                o1 = w["u"]
                kb.tt(o1[:, :, :], X[1][:, :].re("p (h c) -> p h c", h=4), bc3(View(sm, egc.ap.unsqueeze(2)), [128, 4, 128]), ALU.mult)
                kb.tt(fl(w["obf"][:, :, :]), fl(o1[:, :, :]), X[2][:, :], ALU.add)
                kb.tt(S[:, :, :], S[:, :, :], bc3(View(sm, egl8.ap.unsqueeze(2)), [128, 8, 128]), ALU.mult)
                kb.tt(fl(S[:, :, :]), fl(S[:, :, :]), XS[:, :], ALU.add)
                kb.copy(Sbf[:, :, :], S[:, :, :], eng=kb.act)
                for h in range(4):
                    for d in range(2):
                        kb.mm((Yp[1], Yp[2])[d][:, h * 64:(h + 1) * 64], w["obf"][HALF[d], h, :], idb[d])
                for d in range(2):
                    o3 = (Yp[1], Yp[2])[d][:, 0:256].re("p (h c) -> p h c", h=4)
                    n = nch[d]
                    if n in visited:
                        kb.tt(oT[:, :, sls[d]], oT[:, :, sls[d]], o3, ALU.add)
                    else:
                        kb.copy(oT[:, :, sls[d]], o3, eng=kb.act)
                        visited.add(n)
            kb.barrier()
            stw.close()
            with ExitStack() as st2:
                z = [kb.sbuf(st2, "gz%d" % i, [128, NT], F32) for i in range(2)]
                sq = [kb.sbuf(st2, "gsq2%d" % i, [128, 512], F32) for i in range(2)]
                rs = [kb.sbuf(st2, "grs2%d" % i, [128, 512], F32) for i in range(2)]
                ob = [kb.sbuf(st2, "gob%d" % i, [128, NT], BF16) for i in range(2)]
                nw = kb.sbuf(st2, "gnw", [128, 1], F32)
                kb.dma(kb.sp, nw, G["gdn_norm_w"][l].re("(p o) -> p o", o=1))

                def head_g(h):
                    z_, sq_, rs_, ob_, ps_ = z[h % 2], sq[h % 2], rs[h % 2], ob[h % 2], Yp[h % 2]
                    kb.dma(kb.sp, z_, C.pT[O_Z // 128 + h])
                    yield
                    kb.actf(z_[:, :], z_[:, :], AF.Silu)
                    yield
                    for (t0, tl) in TBLK:
                        x = oT[:, h, t0:t0 + tl]
                        kb.tt(sq_[:, 0:tl], x, x, ALU.mult)
                        yield
                        kb.mm(ps_[:, 0:tl], C.onesm[:, :], sq_[:, 0:tl])
                        yield
                        kb.ts(rs_[:, 0:tl], ps_[:, 0:tl], EPS, None, ALU.add)
                        yield
                        kb.actf(rs_[:, 0:tl], rs_[:, 0:tl], AF.Sqrt)
                        yield
                        kb.recip(rs_[:, 0:tl], rs_[:, 0:tl])
                        yield
                        kb.stt(sq_[:, 0:tl], x, nw[:, 0:1], rs_[:, 0:tl], ALU.mult, ALU.mult)
                        yield
                        kb.tt(ob_[:, t0:t0 + tl], sq_[:, 0:tl], z_[:, t0:t0 + tl], ALU.mult)
                        yield
                    kb.dma(kb.sp, C.brT[0][h], ob_)
                    yield
                for h0 in (0, 2):
                    lockstep([head_g(h0), head_g(h0 + 1)])
                kb.barrier()


def phase_gdn(kb, C, G, l):
    with ExitStack() as st0:
        GD = gdn_alloc(kb, st0, C, G)
        with ExitStack() as st:
            for _ in gdn_stepA_g(kb, C, G, l, GD, st):
                pass
            kb.barrier()
        gdn_stepBC(kb, C, G, l, GD)


def declare_inputs(kb):
    G = {}

    def inp(name, shape, dt=F32):
        G[name] = kb.dram(name, shape, dt, kind="ExternalInput")

    L = 2
    inp("x", [2048, D]); inp("ctx", [256, D]); inp("cT", [128, 8]); inp("cctxT", [128, 8])
    inp("pos", [2048, D]); inp("ident", [128, 128])
    inp("w_mod", [L, D, 6 * D]); inp("b_modT", [L, 128, 48])
    inp("w_inR", [L, NCH, 128, 8, 128]); inp("b_inT", [L, 128, NCH])
    inp("gdn_cwT", [L, 128, 12, 5]); inp("gdn_ab", [L, 8, 2]); inp("gdn_norm_w", [L, 128])
    inp("conf_wT", [L, 128, 4, 31]); inp("conf_vT", [L, 128, 3, 4])
    inp("sgu_vT", [L, 128, 2, 4]); inp("sgu_wsT", [L, 4, 128, 128]); inp("sgu_bs", [L, 4, 128])
    inp("w_branch", [L, 4, 512, D]); inp("w_out", [L, D, D])
    for nm in ("b_out", "ln1_g", "ln1_b", "ln2_g", "ln2_b"):
        inp(nm, [L, D])
    inp("rw", [L, D, 20]); inp("rb", [L, 20])
    inp("w_gate", [L, 16, D, 512]); inp("w_up", [L, 16, D, 512]); inp("w_down", [L, 16, 512, D])
    inp("dft_c", [128, 256]); inp("dft_lat", [2, 2048, 2048]); inp("dft_ctx", [2, 256, 256])
    inp("gmask", [64, 6, 64]); inp("gm2", [128, 3, 64]); inp("gml", [128, 5, 128])
    return G


FUSE_INPROJ_GDNA = False


def build(layers=(0, 1), first=True, last=True, debug=False, phases=None):
    nc = bass.Bass("TRN2", target_bir_lowering=False)
    kb = KB(nc)
    G = declare_inputs(kb)
    C = Ctx()
    kind = "ExternalOutput" if debug else "Internal"
    if first and last:
        C.h = [kb.dram("h%d" % t, [128, D], F32, kind=kind) for t in range(NTILE)]
    else:
        C.h = [kb.dram("h%d" % t, [128, D], F32, kind="ExternalOutput") for t in range(NTILE)]
        if not first:
            C.hin = [kb.dram("hin%d" % t, [128, D], F32, kind="ExternalInput") for t in range(NTILE)]
    C.pT = [kb.dram("pT%d" % c, [128, NT], F32, kind=kind) for c in range(NCH)]
    C.brT = [[kb.dram("brT%d_%d" % (i, c), [128, NT], BF16, kind=kind) for c in range(4)] for i in range(4)]
    C.oT = [kb.dram("oT%d" % c, [128, NT], F32, kind=kind) for c in range(4)]
    G["out"] = kb.dram("out", [2048, D], F32, kind="ExternalOutput")
    with ExitStack() as gst:
        C.gst = gst
        phase_consts(kb, C, G)
        mod_done = set()
        if first and (phases is None):
            phase_h0_mod0(kb, C, G)
            mod_done.add(layers[0])
        elif first:
            phase_h0(kb, C, G)
        else:
            with ExitStack() as st:
                tmp = [kb.sbuf(st, "hcp%d" % i, [128, D], F32) for i in range(2)]
                for t in range(NTILE):
                    kb.dma(kb.sp, tmp[t % 2], C.hin[t])
                    kb.dma(kb.sp, C.h[t], tmp[t % 2])
                kb.barrier()
        for l in layers:
            is_last = last and (l == layers[-1])

            def on(p):
                return phases is None or p in phases
            C.modT, C.onep = C.modT_all[l], C.onep_all[l]
            if on("mod") and l not in mod_done:
                with ExitStack() as stm:
                    for _ in phase_mod_g(kb, C, G, l, stm):
                        pass
                    kb.barrier()
            if FUSE_INPROJ_GDNA and on("inproj") and on("gdn"):
                with ExitStack() as sg0:
                    GD = gdn_alloc(kb, sg0, C, G)
                    phase_inproj_gdnA(kb, C, G, l, GD)
                    gdn_stepBC(kb, C, G, l, GD)
            else:
                if on("inproj"):
                    with ExitStack() as st:
                        xT = kb.sbuf(st, "xT1", [128, 8, NT], BF16)
                        phase_inproj(kb, C, G, l, xT)
                if on("gdn"):
                    phase_gdn(kb, C, G, l)
            li = list(layers).index(l)
            if on("conf") and on("sgu") and phases is None and li + 1 < len(layers):
                phase_conf_sgu_mod(kb, C, G, l, layers[li + 1])
                mod_done.add(layers[li + 1])
            elif on("conf") and on("sgu"):
                phase_conf_sgu(kb, C, G, l)
            else:
                if on("conf"):
                    phase_conformer(kb, C, G, l)
                if on("sgu"):
                    phase_sgu(kb, C, G, l)
            if on("fft"):
                phase_fft(kb, C, G, l)
            if on("merge"):
                phase_merge(kb, C, G, l, is_last)
            if on("moe"):
                phase_moe(kb, C, G, l, is_last)
        kb.finish([G["out"]] + C.h)
    return nc, kb


def _consts():
    c = {}
    rows = 2048 // 64
    row = np.broadcast_to(np.arange(rows, dtype=np.float32)[:, None], (rows, 64)).reshape(-1)
    col = np.broadcast_to(np.arange(64, dtype=np.float32)[None, :], (rows, 64)).reshape(-1)
    quarter = D // 4
    omega = (1.0 / (10000.0 ** (np.arange(quarter, dtype=np.float32) / np.float32(quarter)))).astype(np.float32)

    def enc(pos):
        ang = (pos[:, None] * omega[None, :]).astype(np.float32)
        return np.concatenate([np.sin(ang), np.cos(ang)], -1)
    c["pos"] = np.concatenate([enc(row), enc(col)], -1).astype(np.float32)
    c["ident"] = np.eye(128, dtype=np.float32)

    def dft(n):
        k = np.arange(n, dtype=np.int64)
        ang = 2.0 * np.pi * ((k[:, None] * k[None, :]) % n).astype(np.float64) / n
        s = 1.0 / np.sqrt(n)
        return (np.cos(ang) * s).astype(np.float32), (np.sin(ang) * s).astype(np.float32)
    cc, sc = dft(128)
    c["dft_c"] = np.concatenate([cc, sc], 1)
    ct, stt = dft(2048)
    c["dft_lat"] = np.stack([ct, -stt])
    ct, stt = dft(256)
    c["dft_ctx"] = np.stack([ct, -stt])
    i = np.arange(64)
    k_, j_ = i[:, None], i[None, :]
    m = np.stack([(k_ <= j_), (k_ > j_), (k_ >= j_), (k_ < j_), (k_ == j_), (k_ != j_)], 1).astype(np.float32)
    c["gmask"] = np.ascontiguousarray(m)
    lo, up, le, ge, eye = (k_ > j_), (k_ < j_), (k_ <= j_), (k_ >= j_), (k_ == j_)
    gm2 = np.stack([np.concatenate([lo, up], 0), np.concatenate([le, ge], 0), np.concatenate([eye, eye], 0)], 1)
    c["gm2"] = np.ascontiguousarray(gm2.astype(np.float32))

    def bd(a, b):
        z = np.zeros((128, 128), np.float32)
        z[:64, :64] = a
        z[64:, 64:] = b
        return z
    one = np.ones((64, 64), np.float32)
    self_f = np.zeros((128, 128), np.float32); self_f[:64, :] = 1.0
    self_b = np.zeros((128, 128), np.float32); self_b[64:, :] = 1.0
    c["gml"] = np.ascontiguousarray(np.stack([bd(le, ge), bd(lo, up), bd(one, one), self_f, self_b], 1).astype(np.float32))
    return c


_CONSTS = None
PERM = np.concatenate([np.arange(0, 2048), np.arange(2064, 8720), np.arange(2048, 2064)])


def prep_shared(inp):
    global _CONSTS
    if _CONSTS is None:
        _CONSTS = _consts()
    f = lambda a: np.ascontiguousarray(np.asarray(a, dtype=np.float32))
    S = dict(_CONSTS)
    L = 2
    S["w_mod"] = f(inp["w_mod"])
    S["b_modT"] = f(np.asarray(inp["b_mod"]).reshape(L, 48, 128).transpose(0, 2, 1))
    w = np.asarray(inp["w_in"])[:, :, PERM]
    wp = np.zeros((L, D, NCH * 128), np.float32)
    wp[:, :, :8720] = w
    S["w_inR"] = f(wp.reshape(L, 8, 128, NCH, 128).transpose(0, 3, 2, 1, 4))
    b = np.zeros((L, NCH * 128), np.float32)
    b[:, :8720] = np.asarray(inp["b_in"])[:, PERM]
    S["b_inT"] = f(b.reshape(L, NCH, 128).transpose(0, 2, 1))
    S["gdn_cwT"] = f(np.asarray(inp["gdn_conv_w"]).reshape(L, 5, 12, 128).transpose(0, 3, 2, 1))
    S["gdn_ab"] = f(np.stack([np.asarray(inp["gdn_a_log"]).reshape(L, 8), np.asarray(inp["gdn_dt_bias"]).reshape(L, 8)], -1))
    S["gdn_norm_w"] = f(inp["gdn_norm_w"])
    S["conf_wT"] = f(np.asarray(inp["conf_dw_w"]).reshape(L, 31, 4, 128).transpose(0, 3, 2, 1))
    S["conf_vT"] = f(np.stack([np.asarray(inp[k]).reshape(L, 4, 128) for k in ("conf_dw_b", "conf_ln_g", "conf_ln_b")], 1).transpose(0, 3, 1, 2))
    S["sgu_vT"] = f(np.stack([np.asarray(inp[k]).reshape(L, 4, 128) for k in ("sgu_ln_g", "sgu_ln_b")], 1).transpose(0, 3, 1, 2))
    S["sgu_wsT"] = f(np.asarray(inp["sgu_ws"]).transpose(0, 1, 3, 2))
    S["sgu_bs"] = f(inp["sgu_bs"])
    for k in ("w_branch", "w_out", "b_out", "ln1_g", "ln1_b", "ln2_g", "ln2_b"):
        S[k] = f(inp[k])
    S["rw"] = f(np.concatenate([np.asarray(inp["router_group_w"]), np.asarray(inp["router_expert_w"])], -1))
    S["rb"] = f(np.concatenate([np.asarray(inp["router_group_b"]), np.asarray(inp["router_expert_b"])], -1))
    S["w_gate"] = f(inp["expert_w_gate"]); S["w_up"] = f(inp["expert_w_up"]); S["w_down"] = f(inp["expert_w_down"])
    S["cctxT"] = f(np.asarray(inp["c_ctx"]).reshape(8, 128).T)
    return S


def prep_core(inp, S, b):
    m = dict(S)
    m["x"] = np.ascontiguousarray(np.asarray(inp["x"][b], dtype=np.float32))
    m["ctx"] = np.ascontiguousarray(np.asarray(inp["ctx"][b], dtype=np.float32))
    m["cT"] = np.ascontiguousarray(np.asarray(inp["c"][b], dtype=np.float32).reshape(8, 128).T)
    return m


_NC_CACHE = {}


def kernel(**inputs):
    S = prep_shared(inputs)
    maps = [prep_core(inputs, S, b) for b in range(8)]
    if "full" not in _NC_CACHE:
        _NC_CACHE["full"] = build()[0]
    res = run_bass_kernel_spmd(_NC_CACHE["full"], maps, core_ids=list(range(8)))
    return np.stack([np.asarray(r["out"]) for r in res.results], 0).astype(np.float32)
```
